# Optimizing a Trainium2 kernel written in Bass

```python
import jax, jax.numpy as jnp
from jax import lax
import numpy as np

D_MODEL = 1024
BATCH = 8
SEQ = 2048
DEPTH = 1

CTX_LEN = 256
GRID_W = 64

GDN_HEADS = 8
GDN_DK = 128
GDN_DV = 128
GDN_W = GDN_HEADS * GDN_DV
SHORT_CONV = 5
CHUNK = 64
CONF_W = D_MODEL
CONF_KERNEL = 31
N_EXPERTS = 32
TOP_K = 4
D_FF = D_MODEL
SWIGLU_LIMIT = 7.0
SWIGLU_ALPHA = 1.702
MOE_BLOCK = 128
EPS = 1e-6
POS_BASE = 10000.0
F32 = jnp.float32

V_OFF = GDN_HEADS * GDN_DK
BETA_OFF = V_OFF + GDN_W
ALPHA_OFF = BETA_OFF + 2 * GDN_HEADS
STATE_COLS = ALPHA_OFF + 2 * GDN_HEADS
Q_OFF = STATE_COLS
Z_OFF = Q_OFF + GDN_HEADS * GDN_DK
GLU_OFF = Z_OFF + GDN_W
GATE_OFF = GLU_OFF + 2 * CONF_W
N_IN = GATE_OFF + 2 * D_MODEL

kernel_name = 'hybrid_gdn_conformer_moe_dit'


def rms_norm(x, g):
    xf = x.astype(F32)
    y = xf * lax.rsqrt(jnp.mean(xf * xf, axis=-1, keepdims=True) + EPS)
    return (y * g.astype(F32)).astype(x.dtype)


def layer_norm(x, g, b):
    xf = x.astype(F32)
    mu = jnp.mean(xf, axis=-1, keepdims=True)
    var = jnp.mean(jnp.square(xf - mu), axis=-1, keepdims=True)
    y = (xf - mu) * lax.rsqrt(var + EPS) * g.astype(F32) + b.astype(F32)
    return y.astype(x.dtype)


def l2_normalize(x):
    return x * lax.rsqrt(jnp.sum(x * x, axis=-1, keepdims=True) + EPS)


def depthwise_conv(x, w):
    width = w.shape[0]
    return lax.conv_general_dilated(
        x, w[:, None, :].astype(x.dtype), window_strides=(1,),
        padding=[(width // 2, width // 2)],
        dimension_numbers=('NWC', 'WIO', 'NWC'),
        feature_group_count=x.shape[-1])


def grid_pos_embedding(rows, dtype):
    row = jnp.repeat(jnp.arange(rows, dtype=F32), GRID_W)
    col = jnp.tile(jnp.arange(GRID_W, dtype=F32), rows)
    quarter = D_MODEL // 4
    omega = POS_BASE ** (-jnp.arange(quarter, dtype=F32) / quarter)
    def emb(p):
        ang = p[:, None] * omega[None, :]
        return jnp.concatenate([jnp.sin(ang), jnp.cos(ang)], axis=-1)
    return jnp.concatenate([emb(row), emb(col)], axis=-1).astype(dtype)


def modulation(cond, w_mod, b_mod):
    m = jax.nn.silu(cond) @ w_mod + b_mod
    return jnp.split(m[..., None, :], 6, axis=-1)


def gdn_key_value(p_state, conv_kv, a_log, dt_bias):
    b_, l_, _ = p_state.shape
    kv = jax.nn.silu(depthwise_conv(p_state[..., :BETA_OFF], conv_kv)).astype(F32)
    k = l2_normalize(kv[..., :V_OFF].reshape(b_, l_, GDN_HEADS, GDN_DK))
    v = kv[..., V_OFF:].reshape(b_, l_, GDN_HEADS, GDN_DV)
    beta = jax.nn.sigmoid(p_state[..., BETA_OFF:ALPHA_OFF].astype(F32)).reshape(b_, l_, 2, GDN_HEADS)
    alpha = p_state[..., ALPHA_OFF:STATE_COLS].astype(F32).reshape(b_, l_, 2, GDN_HEADS)
    g = -jnp.exp(a_log.astype(F32)) * jax.nn.softplus(alpha + dt_bias.astype(F32))
    return k, v, beta, g


def gdn_query(p_q, conv_q):
    b_, l_, _ = p_q.shape
    q = jax.nn.silu(depthwise_conv(p_q, conv_q)).astype(F32).reshape(b_, l_, GDN_HEADS, GDN_DK)
    return l2_normalize(q) * GDN_DK ** -0.5


def chunk_gated_delta(k, v, beta, g, s0, q=None):
    b_, l_, h_, _ = k.shape
    dv = v.shape[-1]
    n = l_ // CHUNK
    def to_chunks(t):
        return t.reshape(b_, n, CHUNK, h_, -1).transpose(1, 0, 3, 2, 4)
    kc, vc = to_chunks(k), to_chunks(v)
    bc = to_chunks(beta[..., None])[..., 0]
    gc = jnp.cumsum(to_chunks(g[..., None])[..., 0], axis=-1)
    idx = jnp.arange(CHUNK)
    lower_incl = idx[:, None] >= idx[None, :]
    strict = idx[:, None] > idx[None, :]
    decay = jnp.exp(jnp.where(lower_incl, gc[..., :, None] - gc[..., None, :], -jnp.inf))
    k_beta = kc * bc[..., None]
    a_strict = jnp.where(strict, jnp.einsum('nbhik,nbhjk->nbhij', k_beta, kc) * decay, 0.0)
    rhs = jnp.concatenate([vc * bc[..., None], k_beta * jnp.exp(gc)[..., None]], axis=-1)
    sol = lax.linalg.triangular_solve(a_strict, rhs, left_side=True, lower=True, unit_diagonal=True)
    value, k_cum = sol[..., :dv], sol[..., dv:]
    k_tail = kc * jnp.exp(gc[..., -1:] - gc)[..., None]
    chunk_decay = jnp.exp(gc[..., -1])
    xs = (k_tail, value, k_cum, chunk_decay)
    if q is not None:
        qc = to_chunks(q)
        attn = jnp.einsum('nbhik,nbhjk->nbhij', qc, kc) * decay
        xs = xs + (qc, gc, attn)

    def step(state, xs_c):
        k_t, val, kcum, dec = xs_c[:4]
        v_new = val - jnp.einsum('bhck,bhkv->bhcv', kcum, state)
        new_state = state * dec[..., None, None] + jnp.einsum('bhck,bhcv->bhkv', k_t, v_new)
        if q is None:
            return new_state, None
        q_t, g_t, att = xs_c[4:]
        o = (jnp.einsum('bhck,bhkv->bhcv', q_t * jnp.exp(g_t)[..., None], state)
             + jnp.einsum('bhij,bhjv->bhiv', att, v_new))
        return new_state, o

    s_final, o = lax.scan(step, s0, xs)
    if q is None:
        return None, s_final
    return o.transpose(1, 0, 3, 2, 4).reshape(b_, l_, h_, dv), s_final


def gdn_bidirectional(k, v, beta, g, s0_f, s0_b, q=None):
    flip = lambda t: jnp.flip(t, axis=1)
    o_f, s_f = chunk_gated_delta(k, v, beta[:, :, 0], g[:, :, 0], s0_f, q)
    o_b, s_b = chunk_gated_delta(flip(k), flip(v), flip(beta[:, :, 1]), flip(g[:, :, 1]), s0_b,
                                 None if q is None else flip(q))
    o = None if q is None else o_f + flip(o_b)
    return o, s_f, s_b


def mixer(h, mp, s0_f, s0_b):
    (w_in, conv_kv, conv_q, a_log, dt_bias, gdn_norm_g, w_proj_a,
     conf_dw, conf_dw_b, conf_ln_g, conf_ln_b, w_proj_b, w_out) = mp
    b_, l_, _ = h.shape
    proj = h @ w_in
    k, v, beta, g = gdn_key_value(proj[..., :STATE_COLS], conv_kv, a_log, dt_bias)
    q = gdn_query(proj[..., Q_OFF:Z_OFF], conv_q)
    o, s_f, s_b = gdn_bidirectional(k, v, beta, g, s0_f, s0_b, q)
    z = proj[..., Z_OFF:GLU_OFF].astype(F32).reshape(b_, l_, GDN_HEADS, GDN_DV)
    o = (rms_norm(o, gdn_norm_g) * jax.nn.silu(z)).reshape(b_, l_, GDN_W).astype(h.dtype)
    y_a = o @ w_proj_a
    glu = proj[..., GLU_OFF:GATE_OFF]
    u = glu[..., :CONF_W] * jax.nn.sigmoid(glu[..., CONF_W:])
    u = depthwise_conv(u, conf_dw) + conf_dw_b
    u = jax.nn.silu(layer_norm(u, conf_ln_g, conf_ln_b))
    y_b = u @ w_proj_b
    gates = jax.nn.sigmoid(proj[..., GATE_OFF:])
    merged = gates[..., :D_MODEL] * y_a + gates[..., D_MODEL:] * y_b
    return merged @ w_out, s_f, s_b


def moe(h, w_router, b_router, w_gate_up, b_gate_up, w_down, b_down):
    b_, l_, d = h.shape
    n_tok = b_ * l_
    xt = h.reshape(n_tok, d)
    logits = (xt @ w_router).astype(F32) + b_router.astype(F32)
    top_logit, top_idx = lax.top_k(logits, TOP_K)
    top_w = jax.nn.softmax(top_logit, axis=-1)
    n_assign = n_tok * TOP_K
    flat_e = top_idx.reshape(-1)
    order = jnp.argsort(flat_e)
    sorted_e = flat_e[order]
    sorted_tok = (order // TOP_K).astype(jnp.int32)
    sorted_w = top_w.reshape(-1)[order]
    counts = jnp.zeros((N_EXPERTS,), jnp.int32).at[flat_e].add(1)
    padded = (counts + MOE_BLOCK - 1) // MOE_BLOCK * MOE_BLOCK
    start = jnp.cumsum(counts) - counts
    padded_end = jnp.cumsum(padded)
    padded_start = padded_end - padded
    dest = padded_start[sorted_e] + jnp.arange(n_assign, dtype=jnp.int32) - start[sorted_e]
    n_rows = -(-n_assign // MOE_BLOCK) * MOE_BLOCK + N_EXPERTS * MOE_BLOCK
    n_blocks = n_rows // MOE_BLOCK
    row_tok = jnp.full((n_rows,), n_tok, jnp.int32).at[dest].set(sorted_tok)
    row_w = jnp.zeros((n_rows,), F32).at[dest].set(sorted_w)
    block_expert = jnp.minimum(
        jnp.searchsorted(padded_end, jnp.arange(n_blocks, dtype=jnp.int32) * MOE_BLOCK, side='right'),
        N_EXPERTS - 1)
    x_pad = jnp.concatenate([xt, jnp.zeros((1, d), xt.dtype)], axis=0)
    xb = x_pad[row_tok].reshape(n_blocks, MOE_BLOCK, d)

    def expert_block(args):
        xe, e = args
        gu = xe @ w_gate_up[e] + b_gate_up[e]
        gl = jnp.minimum(gu[..., :D_FF], SWIGLU_LIMIT)
        lin = jnp.clip(gu[..., D_FF:], -SWIGLU_LIMIT, SWIGLU_LIMIT)
        act = gl * jax.nn.sigmoid(SWIGLU_ALPHA * gl) * (lin + 1)
        return act @ w_down[e] + b_down[e]

    yb = lax.map(expert_block, (xb, block_expert))
    y = jnp.zeros((n_tok + 1, d), F32).at[row_tok].add(yb.reshape(n_rows, d).astype(F32) * row_w[:, None])
    return y[:n_tok].reshape(b_, l_, d).astype(h.dtype)


def block(x, mod6, norms, mp, ffn_p, s0_f, s0_b):
    shift_m, scale_m, gate_m, shift_f, scale_f, gate_f = mod6
    g_pre_m, g_post_m, g_pre_f, g_post_f = norms
    h = rms_norm(x, g_pre_m) * (1 + scale_m) + shift_m
    y, s_f, s_b = mixer(h, mp, s0_f, s0_b)
    x = x + gate_m * rms_norm(y, g_post_m)
    h = rms_norm(x, g_pre_f) * (1 + scale_f) + shift_f
    x = x + gate_f * rms_norm(moe(h, *ffn_p), g_post_f)
    return x, s_f, s_b


def context_states(ctx, mod6, g_pre_m, mp):
    shift_m, scale_m = mod6[0], mod6[1]
    h = rms_norm(ctx, g_pre_m) * (1 + scale_m) + shift_m
    w_in, conv_kv, a_log, dt_bias = mp[0], mp[1], mp[3], mp[4]
    k, v, beta, g = gdn_key_value(h @ w_in[:, :STATE_COLS], conv_kv, a_log, dt_bias)
    zero = jnp.zeros((ctx.shape[0], GDN_HEADS, GDN_DK, GDN_DV), F32)
    _, s_f, s_b = gdn_bidirectional(k, v, beta, g, zero, zero)
    return s_f, s_b


def setup_inputs(seed: int = 0) -> dict:
    key = jax.random.key(seed)
    ks = jax.random.split(key, 32)
    def nrm(k, shape, scale):
        return jax.random.normal(k, shape, F32) * scale
    def gain(k, shape):
        return 1.0 + 0.05 * jax.random.normal(k, shape, F32)
    dt = jnp.exp(jax.random.uniform(ks[10], (DEPTH, 2, GDN_HEADS), F32, np.log(1e-3), np.log(1e-1)))
    return {
        'x': nrm(ks[0], (BATCH, SEQ, D_MODEL), 1.0),
        'c': nrm(ks[1], (BATCH, D_MODEL), 1.0),
        'ctx': nrm(ks[2], (BATCH, CTX_LEN, D_MODEL), 1.0),
        'c_ctx': nrm(ks[3], (D_MODEL,), 1.0),
        'w_mod': nrm(ks[4], (DEPTH, D_MODEL, 6 * D_MODEL), 0.5 * D_MODEL ** -0.5),
        'b_mod': nrm(ks[5], (DEPTH, 6 * D_MODEL), 0.01),
        'g_pre_mix': gain(ks[6], (DEPTH, D_MODEL)),
        'g_post_mix': gain(ks[7], (DEPTH, D_MODEL)),
        'g_pre_ffn': gain(ks[8], (DEPTH, D_MODEL)),
        'g_post_ffn': gain(ks[9], (DEPTH, D_MODEL)),
        'w_in': nrm(ks[11], (DEPTH, D_MODEL, N_IN), D_MODEL ** -0.5),
        'conv_kv': nrm(ks[12], (DEPTH, SHORT_CONV, 2 * GDN_W), SHORT_CONV ** -0.5),
        'conv_q': nrm(ks[13], (DEPTH, SHORT_CONV, GDN_HEADS * GDN_DK), SHORT_CONV ** -0.5),
        'a_log': jnp.log(jax.random.uniform(ks[14], (DEPTH, 2, GDN_HEADS), F32, 1.0, 16.0)),
        'dt_bias': dt + jnp.log(-jnp.expm1(-dt)),
        'gdn_norm_g': gain(ks[15], (DEPTH, GDN_DV)),
        'w_proj_a': nrm(ks[16], (DEPTH, GDN_W, D_MODEL), GDN_W ** -0.5),
        'conf_dw': nrm(ks[17], (DEPTH, CONF_KERNEL, CONF_W), CONF_KERNEL ** -0.5),
        'conf_dw_b': nrm(ks[18], (DEPTH, CONF_W), 0.01),
        'conf_ln_g': gain(ks[19], (DEPTH, CONF_W)),
        'conf_ln_b': nrm(ks[20], (DEPTH, CONF_W), 0.01),
        'w_proj_b': nrm(ks[21], (DEPTH, CONF_W, D_MODEL), CONF_W ** -0.5),
        'w_out': nrm(ks[22], (DEPTH, D_MODEL, D_MODEL), D_MODEL ** -0.5),
        'w_router': nrm(ks[23], (DEPTH, D_MODEL, N_EXPERTS), D_MODEL ** -0.5),
        'b_router': nrm(ks[24], (DEPTH, N_EXPERTS), 0.01),
        'w_gate_up': nrm(ks[25], (DEPTH, N_EXPERTS, D_MODEL, 2 * D_FF), D_MODEL ** -0.5),
        'b_gate_up': nrm(ks[26], (DEPTH, N_EXPERTS, 2 * D_FF), 0.01),
        'w_down': nrm(ks[27], (DEPTH, N_EXPERTS, D_FF, D_MODEL), D_FF ** -0.5),
        'b_down': nrm(ks[28], (DEPTH, N_EXPERTS, D_MODEL), 0.01),
    }


def reference(x, c, ctx, c_ctx, w_mod, b_mod, g_pre_mix, g_post_mix, g_pre_ffn, g_post_ffn,
              w_in, conv_kv, conv_q, a_log, dt_bias, gdn_norm_g, w_proj_a,
              conf_dw, conf_dw_b, conf_ln_g, conf_ln_b, w_proj_b, w_out,
              w_router, b_router, w_gate_up, b_gate_up, w_down, b_down):
    rows = x.shape[1] // GRID_W
    x = x + grid_pos_embedding(rows, x.dtype)[None]
    for layer in range(DEPTH):
        mod_lat = modulation(c, w_mod[layer], b_mod[layer])
        mod_ctx = modulation(c_ctx, w_mod[layer], b_mod[layer])
        norms = (g_pre_mix[layer], g_post_mix[layer], g_pre_ffn[layer], g_post_ffn[layer])
        mp = (w_in[layer], conv_kv[layer], conv_q[layer], a_log[layer], dt_bias[layer],
              gdn_norm_g[layer], w_proj_a[layer], conf_dw[layer], conf_dw_b[layer],
              conf_ln_g[layer], conf_ln_b[layer], w_proj_b[layer], w_out[layer])
        ffn_p = (w_router[layer], b_router[layer], w_gate_up[layer], b_gate_up[layer],
                 w_down[layer], b_down[layer])
        if layer + 1 < DEPTH:
            zero = jnp.zeros((ctx.shape[0], GDN_HEADS, GDN_DK, GDN_DV), F32)
            ctx_next, s_f, s_b = block(ctx, mod_ctx, norms, mp, ffn_p, zero, zero)
        else:
            s_f, s_b = context_states(ctx, mod_ctx, norms[0], mp)
            ctx_next = ctx
        x, _, _ = block(x, mod_lat, norms, mp, ffn_p, s_f, s_b)
        ctx = ctx_next
    return x
```

```python
import numpy as np
import ml_dtypes
from contextlib import ExitStack
import concourse.bass as bass
import concourse.mybir as mybir
from concourse.bass_utils import run_bass_kernel_spmd

F32 = mybir.dt.float32
BF16 = mybir.dt.bfloat16
ALU = mybir.AluOpType
AF = mybir.ActivationFunctionType

EPS = 1e-6
BIG = 30000.0
NDMA_SEM = 12


class Sched:
    CE = ('pe', 'act', 'dve', 'pool')

    def __init__(self):
        self.ops = []
        self.lw = {}
        self.rd = {}
        self.last_eng = {}
        self.last_dma = {}
        self.ndq = {}
        self.bdeps = set()

    def barrier(self):
        self.bdeps = set(self.last_eng.values()) | set(self.last_dma.values())

    def add(self, eng, fn, r=(), w=(), dma=False):
        i = len(self.ops)
        psr = [k for k in r if k == 'pT' or (isinstance(k, tuple) and k[0] == 'pb')]
        if psr:
            r = [k for k in r if k not in psr]
            w = list(w) + psr
        deps = set()
        for k in r:
            d = self.lw.get(k)
            if d is not None:
                deps.add(d)
        for k in w:
            d = self.lw.get(k)
            if d is not None:
                deps.add(d)
            for d in self.rd.get(k, ()):
                deps.add(d)
        deps |= self.bdeps
        deps.discard(i)
        if dma:
            n = self.ndq.get(eng, 0)
            self.ndq[eng] = n + 1
            self.last_dma[(eng, n % NDMA_SEM)] = i
        else:
            self.last_eng[eng] = i
        self.ops.append(dict(eng=eng, fn=fn, deps=deps, dma=dma, hasdep=False))
        for k in r:
            self.rd.setdefault(k, []).append(i)
        for k in w:
            self.lw[k] = i
            self.rd[k] = []
        return i

    def plan(self):
        ops = self.ops
        for op in ops:
            for d in op['deps']:
                dop = ops[d]
                if dop['eng'] == 'pe' and op['eng'] == 'pe' and not dop['dma'] and not op['dma']:
                    continue
                dop['hasdep'] = True
        cnt = {e: 0 for e in self.CE}
        ndma = {}
        know = {}
        for op in ops:
            e = op['eng']
            kn = know.setdefault(e, {})
            waits = {}
            for d in sorted(op['deps']):
                dop = ops[d]
                if dop['eng'] == 'pe' and e == 'pe' and not dop['dma'] and not op['dma']:
                    continue
                sem, val = dop['tok']
                if kn.get(sem, 0) >= val:
                    continue
                waits[sem] = max(waits.get(sem, 0), val)
                kn[sem] = val
                for s2, v2 in dop['know'].items():
                    if kn.get(s2, 0) < v2:
                        kn[s2] = v2
            if op['dma']:
                n = ndma.get(e, 0)
                ndma[e] = n + 1
                slot = n % NDMA_SEM
                prev = 16 * (n // NDMA_SEM)
                sem = ('dma', e, slot)
                if prev > 0 and kn.get(sem, 0) < prev:
                    waits[sem] = max(waits.get(sem, 0), prev)
                    kn[sem] = prev
                op['tok'] = (sem, prev + 16)
                op['know'] = {k: v for k, v in kn.items() if k in self.CE}
            else:
                if op['hasdep']:
                    cnt[e] += 1
                    op['tok'] = (e, cnt[e])
                    op['know'] = {k: v for k, v in kn.items() if k in self.CE}
                    op['know'][e] = cnt[e]
                else:
                    op['tok'] = None
            op['waits'] = waits
        self.cnt = cnt
        self.ndma = ndma

    def emit(self, nc, es):
        self.plan()
        sems = {}
        for e in self.CE:
            sems[e] = es.enter_context(nc.semaphore("s_" + e))
        for q in self.ndma:
            for s in range(NDMA_SEM):
                sems[('dma', q, s)] = es.enter_context(nc.semaphore("d_%s_%d" % (q, s)))
        ops = self.ops

        def run(ename, eng):
            for op in ops:
                if op['eng'] != ename:
                    continue
                for sem, val in op['waits'].items():
                    eng.wait_ge(sems[sem], val)
                ins = op['fn'](eng)
                tok = op['tok']
                if tok is not None:
                    ins.then_inc(sems[tok[0]], 16 if op['dma'] else 1)
            if ename in self.ndma:
                n = self.ndma[ename]
                for s in range(min(n, NDMA_SEM)):
                    last = 16 * ((n - 1 - s) // NDMA_SEM + 1)
                    eng.wait_ge(sems[('dma', ename, s)], last)

        with nc.Block() as block:
            @block.tensor
            def _(pe):
                run('pe', pe)

            @block.scalar
            def _(act):
                run('act', act)

            @block.vector
            def _(dve):
                run('dve', dve)

            @block.gpsimd
            def _(pool):
                run('pool', pool)

            @block.sync
            def _(sp):
                run('sp', sp)


class Bld:
    def __init__(self, nc, S):
        self.nc = nc
        self.S = S

    def mm(self, out, lhsT, rhs, start=True, stop=True, r=(), w=()):
        return self.S.add('pe', lambda e: e.matmul(out, lhsT, rhs, start=start, stop=stop), r, w)

    def tr(self, out, in_, ident, r=(), w=()):
        return self.S.add('pe', lambda e: e.transpose(out, in_, ident), r, w)

    def act(self, out, in_, func, bias=None, scale=None, accum_out=None, r=(), w=()):
        kw = {}
        if bias is not None:
            kw['bias'] = bias
        if scale is not None:
            kw['scale'] = scale
        if accum_out is not None:
            kw['accum_out'] = accum_out
        return self.S.add('act', lambda e: e.activation(out, in_, func, **kw), r, w)

    def ts(self, eng, out, in0, s1, s2, op0, op1=None, accum_out=None, r=(), w=()):
        kw = {}
        if op1 is not None:
            kw['op1'] = op1
        if accum_out is not None:
            kw['accum_out'] = accum_out
        return self.S.add(eng, lambda e: e.tensor_scalar(out, in0, s1, s2, op0, **kw), r, w)

    def tt(self, eng, out, in0, in1, op, r=(), w=()):
        return self.S.add(eng, lambda e: e.tensor_tensor(out, in0, in1, op), r, w)

    def stt(self, eng, out, in0, scalar, in1, op0, op1, r=(), w=()):
        return self.S.add(eng, lambda e: e.scalar_tensor_tensor(out, in0, scalar, in1, op0, op1), r, w)

    def cp(self, eng, out, in_, r=(), w=()):
        if eng == 'act':
            return self.S.add('act', lambda e: e.copy(out, in_), r, w)
        return self.S.add(eng, lambda e: e.tensor_copy(out, in_), r, w)

    def memset(self, eng, ap, val, r=(), w=()):
        return self.S.add(eng, lambda e: e.memset(ap, val), r, w)

    def dma(self, q, out, in_, r=(), w=()):
        return self.S.add(q, lambda e: e.dma_start(out, in_), r, w, dma=True)


class Cfg:
    def __init__(s, D=1024, H=8, T=2048, TC=256, E=32, NCORES=8):
        s.D, s.H, s.T, s.TC, s.E, s.NCORES = D, H, T, TC, E, NCORES
        s.DFF = D
        s.V_OFF = H * 128
        s.BETA_OFF = 2 * H * 128
        s.ALPHA_OFF = s.BETA_OFF + 2 * H
        s.STATE = s.ALPHA_OFF + 2 * H
        s.Q_OFF = s.STATE
        s.Z_OFF = s.Q_OFF + H * 128
        s.GLU_OFF = s.Z_OFF + H * 128
        s.GATE_OFF = s.GLU_OFF + 2 * D
        s.NIN = s.GATE_OFF + 2 * D


NCF = 6
NCB = 17


def host_consts():
    i = np.arange(128)
    ident = np.eye(128, dtype=np.float32)
    ones = np.ones((128, 128), np.float32)
    LF = (i[:, None] <= i[None, :]).astype(np.float32)
    LB = (i[:, None] >= i[None, :]).astype(np.float32)
    low_incl = (i[:, None] >= i[None, :])
    PML = np.where(low_incl, 0.0, BIG).astype(np.float32)
    PMU = np.where(low_incl.T, 0.0, BIG).astype(np.float32)
    cf = np.concatenate([ident, ones, LF, LB, PML, PMU], axis=1)
    mls = []
    for l in range(7):
        s = 1 << l
        m = ((i[:, None] // (2 * s) == i[None, :] // (2 * s)) & (i[:, None] % (2 * s) >= s)
             & (i[None, :] % (2 * s) < s)).astype(np.float32)
        mls.append(m)
    cb = [ones] + mls + [m.T for m in mls] + [-mls[0], -mls[0].T]
    cb = np.concatenate(cb, axis=1).astype(ml_dtypes.bfloat16)
    return cf, cb


def build(cfg, STOP=99):
    D, H, T, TC, E, DFF = cfg.D, cfg.H, cfg.T, cfg.TC, cfg.E, cfg.DFF
    KD = D // 128
    KF = DFF // 128
    NT = T // 128
    NTC = TC // 128
    NIN = cfg.NIN
    H2 = 2 * H
    nc = bass.Bass("TRN2", target_bir_lowering=False)

    def din(name, shape, dt=F32):
        return nc.dram_tensor(name, list(shape), dt, kind="ExternalInput").ap()

    x_d = din("x", [T, D]); ctx_d = din("ctx", [TC, D]); pos_d = din("pos", [T, D])
    csT_d = din("csT", [128, KD * 2]); wmod_d = din("w_mod", [D, 6 * D]); bmod_d = din("b_mod2", [2, 6 * D])
    gv_d = din("gvecs", [4, D]); win_d = din("w_in", [D, NIN]); convw_d = din("convw", [128, 3 * H * 5])
    ab_d = din("ab", [2, H2]); gdng_d = din("gdn_g", [1, 128])
    wpa_d = din("w_proj_a", [D, D]); wpb_d = din("w_proj_b", [D, D]); wout_d = din("w_out", [D, D])
    cdw_d = din("cdw", [128, KD * 31]); cdv_d = din("cdv", [128, KD * 3])
    wr_d = din("w_router", [D, E]); br_d = din("b_router", [1, E])
    wgu_d = din("w_gate_up", [E, D, 2 * DFF]); bgu_d = din("bgu", [128, E * 2 * KF])
    wd_d = din("w_down", [E, DFF, D]); bd_d = din("b_down", [E, D])
    cf_d = din("cf32", [128, NCF * 128]); cb_d = din("cb16", [128, NCB * 128], BF16)
    out_d = nc.dram_tensor("out", [T, D], F32, kind="ExternalOutput").ap()
    mod_d = nc.dram_tensor("mod_scr", [2, 6 * D], F32).ap()
    x2_d = nc.dram_tensor("x2_scr", [T, D], F32).ap()

    S = Sched()
    b = Bld(nc, S)
    es = ExitStack()
    with es:
        def sbt(name, shape, dt):
            return es.enter_context(nc.sbuf_tensor("s_" + name, list(shape), dt))

        def pst(name, shape, dt):
            return es.enter_context(nc.psum_tensor(name, list(shape), dt))

        R_HT = 0
        RSZ = max(8192, KD * T // 2)
        R_B1 = RSZ
        R_B2 = 2 * RSZ
        R_SC = 3 * RSZ
        SCW = 20480
        AW = R_SC + SCW
        AR = sbt("arena", [128, AW], F32)
        O_WR = AW - 10240
        O_STG = O_WR + 3 * 2048
        O_X = R_SC
        O_SST = O_WR - 3072

        def fv(off, n):
            return AR[:, off:off + n]

        def bv(off, nbf):
            return AR[:, off:off + nbf // 2].bitcast(BF16)

        hT = bv(R_HT, KD * T).rearrange("p (k t) -> p k t", k=KD)

        cf = sbt("cf", [128, NCF * 128], F32)
        cb = sbt("cb", [128, NCB * 128], BF16)
        b.dma('sp', cf[:], cf_d, w=['cf'])
        b.dma('sp', cb[:], cb_d, w=['cb'])
        identf = cf[:, 0:128]; onesf = cf[:, 128:256]; LFm = cf[:, 256:384]; LBm = cf[:, 384:512]
        PML = cf[:, 512:640]; PMU = cf[:, 640:768]
        identb = identf
        onesb = cb[:, 0:128]
        MLm = [cb[:, (1 + l) * 128:(2 + l) * 128] for l in range(7)]
        MUm = [cb[:, (8 + l) * 128:(9 + l) * 128] for l in range(7)]
        NML0 = cb[:, 15 * 128:16 * 128]; NMU0 = cb[:, 16 * 128:17 * 128]
        identb_t = sbt("identb", [128, 128], BF16)
        b.cp('dve', identb_t[:], identf, r=['cf'], w=['identb'])
        identb = identb_t[:]
        epst = sbt("epst", [128, 1], F32)
        b.memset('dve', epst[:], EPS, w=['eps'])
        stat = sbt("stat", [128, 4 * max(NT, 2) + 8], F32)
        b.memset('dve', stat[:], 0.0, w=['stat'])
        Mt = [sbt("M%d" % i, [128, max(D, 1024)], F32) for i in range(3)]
        M = [Mt[i][:, 0:D] for i in range(3)]
        gtmp_t = sbt("gtmp", [128, max(D, 1024)], F32)
        gtmp = gtmp_t[:, 0:D]
        junk_t = gtmp_t
        convw = sbt("convw", [128, 3 * H * 5], F32)
        b.dma('sp', convw[:], convw_d, w=['convw'])
        cdw = sbt("cdw", [128, KD * 31], F32); cdv = sbt("cdv", [128, KD * 3], F32)
        b.dma('sp', cdw[:], cdw_d, w=['cdw']); b.dma('sp', cdv[:], cdv_d, w=['cdv'])
        abt = sbt("abt", [128, 2 * H2], F32)
        b.dma('sp', abt[:, 0:H2], ab_d[0].partition_broadcast(128), w=['abt'])
        b.dma('sp', abt[:, H2:2 * H2], ab_d[1].partition_broadcast(128), w=['abt'])
        b.act(abt[:, 0:H2], abt[:, 0:H2], AF.Exp, r=['abt'], w=['abt'])
        b.ts('dve', abt[:, 0:H2], abt[:, 0:H2], -1.0, None, ALU.mult, r=['abt'], w=['abt'])
        gdng = sbt("gdng", [128, 128], F32)
        b.dma('sp', gdng[:], gdng_d[0].partition_broadcast(128), w=['gdng'])
        assert H2 * 128 + H2 * 64 <= 3072
        Sst = fv(O_SST, H2 * 128)
        Sb = bv(O_SST + H2 * 128, H2 * 128)
        bgu = sbt("bgu", [128, E * 2 * KF], F32)
        b.dma('sp', bgu[:], bgu_d, w=['bgu'])
        Wr = sbt("Wr", [128, NT * E], F32)

        pb = [pst("pb%d" % i, [128, 512], F32) for i in range(7)]
        pT = pst("pT", [128, 1024], BF16)

        wr_i = [0]
        NRING = [3]
        RG = dict(base=O_WR, slot=2048, n=3, sbase=O_STG, sslot=2048, pcols=512)
        CASTENG = ['pool']
        stg_i = [0]

        def wload(srcs):
            slot = wr_i[0] % RG['n']
            wr_i[0] += 1
            kc = srcs[0].shape[0] // 128
            piece = bv(RG['base'] + slot * RG['slot'], kc * RG['pcols']).rearrange("p (k n) -> p k n", k=kc)
            c0 = 0
            for src in srcs:
                n = src.shape[1]
                si = stg_i[0] % 2
                stg_i[0] += 1
                st = fv(RG['sbase'] + si * RG['sslot'], kc * n).rearrange("p (k n) -> p k n", k=kc)
                b.dma('sp', st, src.rearrange("(k p) n -> p k n", p=128), w=[('stg', si)])
                b.cp(CASTENG[0], piece[:, :, c0:c0 + n], st, r=[('stg', si)], w=[('wr', slot)])
                c0 += n
            return piece, ('wr', slot)

        def load_bc(dst, row_ap, key, rkeys=()):
            b.dma('sp', dst, row_ap.partition_broadcast(128), r=list(rkeys), w=[key])

        scs = sbt("scs", [128, KD * 2], F32)
        b.dma('sp', scs[:], csT_d, w=['scs'])
        b.act(scs[:], scs[:], AF.Silu, r=['scs'], w=['scs'])
        modsb = AR[0:2, O_X:O_X + 6 * D]
        bmt = AR[0:2, O_X + 6 * D:O_X + 12 * D]
        b.dma('sp', bmt, bmod_d, w=['bmt'])
        for nb in range(6 * D // 512):
            for half in range(2):
                c0 = nb * 512 + half * 256
                si = stg_i[0] % 2
                stg_i[0] += 1
                st = fv(O_STG + si * 2048, KD * 256).rearrange("p (k n) -> p k n", k=KD)
                b.dma('sp', st, wmod_d[:, c0:c0 + 256].rearrange("(k p) n -> p k n", p=128), w=[('stg', si)])
                for k in range(KD):
                    b.mm(pb[0][0:2, half * 256:(half + 1) * 256], scs[:, 2 * k:2 * k + 2], st[:, k, :],
                         start=(k == 0), stop=(k == KD - 1), r=['scs', ('stg', si)], w=[('pb', 0)])
            b.tt('dve', modsb[:, nb * 512:(nb + 1) * 512], pb[0][0:2, :], bmt[:, nb * 512:(nb + 1) * 512], ALU.add,
                 r=[('pb', 0), 'bmt'], w=['modsb'])
        b.dma('sp', mod_d, modsb, r=['modsb'], w=['mod_d'])

        def make_AB(row, jshift, jscale, grow):
            load_bc(M[0][:], mod_d[row, jscale * D:(jscale + 1) * D], 'M0', ['mod_d'])
            load_bc(gtmp[:], gv_d[grow], 'gtmp')
            load_bc(M[1][:], mod_d[row, jshift * D:(jshift + 1) * D], 'M1', ['mod_d'])
            b.stt('dve', M[0][:], M[0][:], 1.0, gtmp[:], ALU.add, ALU.mult, r=['M0', 'gtmp'], w=['M0'])

        def make_G(row, jgate, grow):
            load_bc(M[2][:], mod_d[row, jgate * D:(jgate + 1) * D], 'M2', ['mod_d'])
            load_bc(gtmp[:], gv_d[grow], 'gtmp')
            b.tt('dve', M[2][:], M[2][:], gtmp[:], ALU.mult, r=['M2', 'gtmp'], w=['M2'])

        scol = [0]

        def rstd_of(src_ap, srckeys, width):
            c = scol[0] % (stat.shape[1] // 2)
            scol[0] += 1
            ss = stat[:, 2 * c:2 * c + 1]; rs = stat[:, 2 * c + 1:2 * c + 2]
            junk = junk_t[:, 0:width]
            b.act(junk, src_ap, AF.Square, accum_out=ss, r=list(srckeys) + ['stat'], w=['gtmp', ('st', c)])
            b.act(rs, ss, AF.Ln, bias=epst[:, 0:1], scale=1.0 / width, r=[('st', c), 'eps'], w=[('st', c)])
            b.act(rs, rs, AF.Exp, scale=-0.5, r=[('st', c)], w=[('st', c)])
            return rs, ('st', c)

        O_XIN = R_B1
        O_PIN = O_XIN + 2 * D
        O_TMP = O_PIN + 2 * D
        O_HB = O_TMP + D
        O_HC = O_HB + D // 2
        O_PN_END = O_HC + KD * TC // 2

        def prenorm(src_d, posd, ntiles, dstT, tagbase):
            for t in range(ntiles):
                xi = fv(O_XIN + (t % 2) * D, D)
                b.dma('sp', xi, src_d[t * 128:(t + 1) * 128, :], w=[('xin', t % 2)])
                if posd is not None:
                    pi = fv(O_PIN + (t % 2) * D, D)
                    b.dma('sp', pi, posd[t * 128:(t + 1) * 128, :], w=[('pin', t % 2)])
                    b.tt('pool', xi, xi, pi, ALU.add, r=[('xin', t % 2), ('pin', t % 2)], w=[('xin', t % 2)])
                rs, rk = rstd_of(xi, [('xin', t % 2)], D)
                tmp = fv(O_TMP, D)
                hb = bv(O_HB, D)
                b.stt('dve', tmp, xi, rs, M[0][:], ALU.mult, ALU.mult, r=[('xin', t % 2), rk, 'M0'], w=['tmp'])
                b.tt('dve', hb, tmp, M[1][:], ALU.add, r=['tmp', 'M1'], w=['hb'])
                for k in range(KD):
                    b.tr(pT[:, k * 128:(k + 1) * 128], hb[:, k * 128:(k + 1) * 128], identb, r=['hb', 'identb'], w=['pT'])
                b.cp('act', dstT[:, :, t * 128:(t + 1) * 128], pT[:, 0:KD * 128].rearrange("p (k n) -> p k n", k=KD),
                     r=['pT'], w=[(tagbase, t)])

        def gdn(hsrc, hkey, NCH, latent, O_GD):
            TT = NCH * 128
            TBk = min(512, TT)
            NB = TT // TBk
            o = [O_GD]

            def alloc(n):
                r = o[0]
                o[0] += n
                return r
            O_PAD = alloc((TT + 4) // 2 + 2); O_CV = alloc(2 * 512); O_SQ = alloc(2 * 256); O_T0 = alloc(2 * 512)
            O_RR = alloc(512); O_DG14 = alloc(64)
            O_KT = alloc(TT // 2); O_QT = alloc(TT // 2); O_VT = alloc(TT // 2); O_ZT = alloc(TT // 2)
            O_OACC = alloc(TT)
            NS = NCH * H2
            O_BA = alloc(2 * NS); O_G = alloc(NS); O_BE = alloc(NS); O_GC = alloc(NS)
            O_ETL = alloc(NS); O_BK = alloc(NS); O_TOT = alloc(NS)
            O_EGC = O_BK; O_ECD = O_TOT; O_LNB = O_G; O_NGC = O_BA; O_NGCB = O_BA + NS
            G = min(4, NCH)
            GWd = G * 128
            NGR = NCH // G
            xo = [O_WR + 4096]
            O_SET2 = xo[0]
            xo[0] += 4 * (TT // 2)
            assert xo[0] <= O_WR + 10240

            def abuf():
                if xo[0] + GWd // 2 <= O_WR + 10240:
                    r = xo[0]
                    xo[0] += GWd // 2
                    return r
                return alloc(GWd // 2)
            O_DB = [[abuf() for _ in range(6)] for _ in range(2)]
            O_N4 = abuf(); O_TA = abuf(); O_TB = abuf(); O_UA = abuf(); O_UB = abuf()
            O_VNB = alloc(128)
            assert o[0] <= O_SST, (o[0], O_SST)
            RG.update(base=O_WR, slot=1024, n=2, sbase=O_WR + 2048, sslot=1024, pcols=256)
            hkeys = [hkey(t) for t in range(NCH)]
            pad = bv(O_PAD, TT + 4)
            cvs = [fv(O_CV + i * 512, 512) for i in range(2)]
            sqs = [bv(O_SQ + i * 256, 512) for i in range(2)]
            t0s = [fv(O_T0 + i * 512, 512) for i in range(2)]
            Dgc = [gtmp_t[:, 128 + i * 64:128 + (i + 1) * 64].bitcast(BF16) for i in range(14)] + [bv(O_DG14, 128)]
            pcnt = [0]
            pass
            sets = [dict(kTn=bv(O_KT, TT), qTn=bv(O_QT, TT), vTb=bv(O_VT, TT), szT=bv(O_ZT, TT), i=0),
                    dict(kTn=bv(O_SET2, TT), qTn=bv(O_SET2 + TT // 2, TT), vTb=bv(O_SET2 + TT, TT),
                         szT=bv(O_SET2 + 3 * TT // 2, TT), i=1)]
            rr = fv(O_RR, 512); oacc = fv(O_OACC, TT)
            b.memset('pool', pad, 0.0, w=['pad'])
            wp, wk = wload([win_d[:, cfg.BETA_OFF:cfg.BETA_OFF + 2 * H2]])
            ba = fv(O_BA, 2 * NS).rearrange("p (c n) -> p c n", c=NCH)
            for c in range(NCH):
                for k in range(KD):
                    b.mm(pb[1][:, 0:2 * H2], hsrc[:, k, c * 128:(c + 1) * 128], wp[:, k, 0:2 * H2],
                         start=(k == 0), stop=(k == KD - 1), r=[hkeys[c], wk], w=[('pb', 1)])
                b.cp('act', ba[:, c, :], pb[1][:, 0:2 * H2], r=[('pb', 1)], w=['ba'])
            v3 = lambda off: fv(off, NS).rearrange("p (c n) -> p c n", c=NCH)
            g3 = v3(O_G); be3 = v3(O_BE); gc3 = v3(O_GC); egc3 = v3(O_EGC); etl3 = v3(O_ETL); ecd3 = v3(O_ECD)
            bk3 = v3(O_BK); tot3 = v3(O_TOT)
            b.act(be3, ba[:, :, 0:H2], AF.Exp, scale=-1.0, r=['ba'], w=['be'])
            b.act(be3, be3, AF.Ln, bias=1.0, r=['be'], w=['be'])
            b.act(be3, be3, AF.Exp, scale=-1.0, r=['be'], w=['be'])
            dtb = abt[:, H2:2 * H2]
            nA = abt[:, 0:H2]
            for c in range(NCH):
                b.tt('dve', g3[:, c, :], ba[:, c, H2:2 * H2], dtb, ALU.add, r=['ba', 'abt'], w=['g'])
            b.act(fv(O_G, NS), fv(O_G, NS), AF.Exp, r=['g'], w=['g'])
            b.act(fv(O_G, NS), fv(O_G, NS), AF.Ln, bias=1.0, r=['g'], w=['g'])
            for c in range(NCH):
                b.tt('dve', g3[:, c, :], g3[:, c, :], nA, ALU.mult, r=['g', 'abt'], w=['g'])
            b.mm(pb[1][:, 0:NS], LFm, fv(O_G, NS), r=['g', 'cf'], w=[('pb', 1)])
            b.cp('dve', gc3[:, :, 0:H], pb[1][:, 0:NS].rearrange("p (c n) -> p c n", c=NCH)[:, :, 0:H], r=[('pb', 1)], w=['gc'])
            b.mm(pb[1][:, 0:NS], LBm, fv(O_G, NS), r=['g', 'cf'], w=[('pb', 1)])
            b.cp('dve', gc3[:, :, H:H2], pb[1][:, 0:NS].rearrange("p (c n) -> p c n", c=NCH)[:, :, H:H2], r=[('pb', 1)], w=['gc'])
            b.mm(pb[1][:, 0:NS], onesf, fv(O_G, NS), r=['g', 'cf'], w=[('pb', 1)])
            b.cp('dve', fv(O_TOT, NS), pb[1][:, 0:NS], r=[('pb', 1)], w=['tot'])
            b.act(fv(O_BK, NS), fv(O_GC, NS), AF.Exp, r=['gc'], w=['bk'])
            b.tt('dve', fv(O_BK, NS), fv(O_BE, NS), fv(O_BK, NS), ALU.mult, r=['be', 'bk'], w=['bk'])
            b.tt('dve', fv(O_ETL, NS), fv(O_TOT, NS), fv(O_GC, NS), ALU.subtract, r=['tot', 'gc'], w=['etl'])
            b.act(fv(O_ETL, NS), fv(O_ETL, NS), AF.Exp, r=['etl'], w=['etl'])
            b.act(fv(O_TOT, NS), fv(O_TOT, NS), AF.Exp, r=['tot', 'etl'], w=['tot', 'ecd'])
            b.act(fv(O_LNB, NS), fv(O_BE, NS), AF.Ln, r=['be', 'g', ('pb', 1)], w=['g', 'lnb'])
            b.ts('dve', fv(O_NGC, NS), fv(O_GC, NS), -1.0, None, ALU.mult, r=['gc', 'ba', 'be'], w=['ba', 'ngc'])
            b.stt('dve', fv(O_NGCB, NS), fv(O_LNB, NS), -1.0, fv(O_NGC, NS), ALU.mult, ALU.add, r=['lnb', 'ngc'], w=['ba', 'ngcb'])
            ngc3 = v3(O_NGC); ngcb3 = v3(O_NGCB)
            sckeys = ['gc', 'egc', 'ecd', 'etl', 'bk', 'be']

            tb_ = [0]

            def tmpa(nwords):
                n = (nwords + 63) // 64 * 64
                if tb_[0] % 1024 + n > 1024:
                    tb_[0] = (tb_[0] // 1024 + 1) * 1024
                r = O_TMPS + tb_[0] % 1024
                tb_[0] += n
                return r, [('tmw', (r - O_TMPS) // 64 + i) for i in range(n // 64)]

            fixc = [0]

            def fixa(i):
                par = fixc[0] % 2
                return O_FIX + (par * 9 + i) * 64, [('fix', par, i)]

            def conv5(widx, src_w, func_after):
                eng = 'dve'
                for j in range(5):
                    wcol = convw[:, widx * 5 + j:widx * 5 + j + 1]
                    if j == 0:
                        b.ts(eng, cv, pad[:, 0:TT], wcol, None, ALU.mult, r=['pad', 'convw'], w=['cv'])
                    else:
                        b.stt(eng, cv, pad[:, j:j + TT], wcol, cv, ALU.mult, ALU.add, r=['pad', 'convw', 'cv'], w=['cv'])
                b.act(cv, cv, AF.Silu, r=['cv'], w=['cv'])

            def proj_to_pad(wp, wk, coff):
                for tb in range(NB):
                    for k in range(KD):
                        b.mm(pb[0][:, 0:TBk], wp[:, k, coff:coff + 128], hsrc[:, k, tb * TBk:(tb + 1) * TBk],
                             start=(k == 0), stop=(k == KD - 1),
                             r=[wk] + hkeys[tb * (TBk // 128):(tb + 1) * (TBk // 128)], w=[('pb', 0)])
                    b.cp('act', pad[:, 2 + tb * TBk:2 + (tb + 1) * TBk], pb[0][:, 0:TBk], r=[('pb', 0)], w=['pad'])

            def l2n(dst, scale):
                b.act(sq, cv, AF.Square, r=['cv'], w=['pad'])
                for tb in range(NB):
                    b.mm(pb[1][:, 0:TBk], onesb, sq[:, tb * TBk:(tb + 1) * TBk], r=['pad', 'cb'], w=[('pb', 1)])
                    b.act(rr[:, 0:TBk], pb[1][:, 0:TBk], AF.Sqrt, bias=epst[:, 0:1], r=[('pb', 1), 'eps'], w=['pad'])
                    b.S.add('dve', lambda e, TBk=TBk: e.reciprocal(rr[:, 0:TBk], rr[:, 0:TBk]), ['pad'], ['pad'])
                    b.stt('dve', dst[:, tb * TBk:(tb + 1) * TBk], cv[:, tb * TBk:(tb + 1) * TBk], scale, rr[:, 0:TBk],
                          ALU.mult, ALU.mult, r=['cv', 'pad'], w=[dst_key[0]])

            v3g = lambda ap: ap.rearrange("p (g n) -> p g n", g=G)
            bcm = lambda m: m.unsqueeze(1).to_broadcast([128, G, 128])
            dg4 = Mt[0][:, 0:GWd]; ndg4 = Mt[0][:, 512:512 + GWd]; ndgb4 = Mt[1][:, 0:GWd]
            m1b = Mt[1][:, 512:1024].bitcast(BF16)
            E4 = m1b[:, 0:GWd]; E24 = m1b[:, 512:512 + GWd]
            m2b = Mt[2][:, 0:1024].bitcast(BF16)
            Eg4 = m2b[:, 0:GWd]; Kb4 = m2b[:, 512:512 + GWd]; A4 = m2b[:, 1024:1024 + GWd]; AT4 = m2b[:, 1536:1536 + GWd]
            N4 = bv(O_N4, GWd); Ta = bv(O_TA, GWd); Tb_ = bv(O_TB, GWd); Ua = bv(O_UA, GWd); Ub = bv(O_UB, GWd)
            vnbs = [bv(O_VNB + i * 64, 128) for i in range(2)]
            vcnt = [0]

            def stageA(h, d, gi, par):
                bs = sets[h % 2]
                kTn = bs['kTn']; qTn = bs['qTn']; vTb = bs['vTb']
                kK = ('kTn', h % 2); kQ = ('qTn', h % 2); kV = ('vTb', h % 2)
                hd = d * H + h
                c0 = gi * G
                PMA = PML if d == 0 else PMU
                PMT = PMU if d == 0 else PML
                MA = MLm if d == 0 else MUm
                NMA0 = NML0 if d == 0 else NMU0
                NMT0 = NMU0 if d == 0 else NML0
                csl = slice(c0 * 128, (c0 + G) * 128)
                bcs = lambda t3: t3[:, c0:c0 + G, hd:hd + 1].to_broadcast([128, G, 128])
                Kt4 = bv(O_DB[par][0], GWd); Vb4 = bv(O_DB[par][1], GWd); U4 = bv(O_DB[par][2], GWd)
                nkc4 = bv(O_DB[par][3], GWd); attnT4 = bv(O_DB[par][4], GWd); qeT4 = bv(O_DB[par][5], GWd)
                kD = lambda i: ('db', par, i)
                JS = [slice(j * 128, (j + 1) * 128) for j in range(G)]
                CS = [slice((c0 + j) * 128, (c0 + j + 1) * 128) for j in range(G)]
                for j in range(G):
                    b.tr(pT[:, JS[j]], kTn[:, CS[j]], identb, r=[kK, 'identb'], w=['pT'])
                    b.tr(pT[:, 512 + j * 128:512 + (j + 1) * 128], vTb[:, CS[j]], identb, r=[kV, 'identb'], w=['pT'])
                b.tt('dve', v3g(Kb4), v3g(pT[:, 0:GWd]), bcs(bk3), ALU.mult, r=['pT', 'bk'], w=['Kb4'])
                b.tt('dve', v3g(Kt4), v3g(pT[:, 0:GWd]), bcs(etl3), ALU.mult, r=['pT', 'etl'], w=[kD(0)])
                b.tt('dve', v3g(Vb4), v3g(pT[:, 512:512 + GWd]), bcs(be3), ALU.mult, r=['pT', 'be'], w=[kD(1)])
                yield
                b.tt('pool', v3g(dg4), bcm(identf), bcs(gc3), ALU.mult, r=['cf', 'gc'], w=['dg4'])
                b.tt('pool', v3g(ndgb4), bcm(identf), bcs(ngcb3), ALU.mult, r=['cf', 'ngcb'], w=['ndgb4'])
                for j in range(G):
                    b.mm(pb[1][:, JS[j]], onesf, dg4[:, JS[j]], start=True, stop=False, r=['cf', 'dg4'], w=[('pb', 1)])
                    b.mm(pb[1][:, JS[j]], ndgb4[:, JS[j]], onesf, start=False, stop=False, r=['cf', 'ndgb4'], w=[('pb', 1)])
                    b.mm(pb[1][:, JS[j]], identf, PMA, start=False, stop=True, r=['cf'], w=[('pb', 1)])
                b.act(E4, pb[1][:, 0:GWd], AF.Exp, scale=-1.0, r=[('pb', 1)], w=['E4'])
                for j in range(G):
                    b.mm(pb[4][:, JS[j]], kTn[:, CS[j]], kTn[:, CS[j]], r=[kK], w=[('pb', 4)])
                b.tt('dve', A4, pb[4][:, 0:GWd], E4, ALU.mult, r=[('pb', 4), 'E4'], w=['A4'])
                if latent:
                    b.tt('pool', v3g(ndg4), bcm(identf), bcs(ngc3), ALU.mult, r=['cf', 'ngc'], w=['ndg4'])
                    for j in range(G):
                        b.mm(pb[2][:, JS[j]], onesf, ndg4[:, JS[j]], start=True, stop=False, r=['cf', 'ndg4'], w=[('pb', 2)])
                        b.mm(pb[2][:, JS[j]], dg4[:, JS[j]], onesf, start=False, stop=False, r=['cf', 'dg4'], w=[('pb', 2)])
                        b.mm(pb[2][:, JS[j]], identf, PMT, start=False, stop=True, r=['cf'], w=[('pb', 2)])
                    b.act(E24, pb[2][:, 0:GWd], AF.Exp, scale=-1.0, r=[('pb', 2)], w=['E24'])
                    for j in range(G):
                        b.mm(pb[3][:, JS[j]], onesf, dg4[:, JS[j]], r=['cf', 'dg4'], w=[('pb', 3)])
                    b.act(Eg4, pb[3][:, 0:GWd], AF.Exp, r=[('pb', 3)], w=['Eg4'])
                    for j in range(G):
                        b.mm(pb[5][:, JS[j]], kTn[:, CS[j]], qTn[:, CS[j]], r=[kK, kQ], w=[('pb', 5)])
                    b.tt('dve', attnT4, pb[5][:, 0:GWd], E24, ALU.mult, r=[('pb', 5), 'E24'], w=[kD(4)])
                    b.tt('pool', qeT4, qTn[:, csl], Eg4, ALU.mult, r=[kQ, 'Eg4'], w=[kD(5)])
                yield
                for j in range(G):
                    b.tr(pT[:, JS[j]], A4[:, JS[j]], identb, r=['A4', 'identb'], w=['pT'])
                b.cp('act', AT4, pT[:, 0:GWd], r=['pT'], w=['AT4'])
                b.tt('pool', v3g(Ta), v3g(A4), bcm(NMA0), ALU.mult, r=['A4', 'cb'], w=['Ta'])
                b.tt('pool', v3g(Ta), v3g(Ta), bcm(identb), ALU.add, r=['Ta', 'identb'], w=['Ta'])
                b.tt('pool', v3g(Ua), v3g(AT4), bcm(NMT0), ALU.mult, r=['AT4', 'cb'], w=['Ua'])
                b.tt('pool', v3g(Ua), v3g(Ua), bcm(identb), ALU.add, r=['Ua', 'identb'], w=['Ua'])
                yield
                Tc, kTc, Tn, kTn_ = Ta, 'Ta', Tb_, 'Tb'
                Uc, kUc, Un, kUn = Ua, 'Ua', Ub, 'Ub'
                for l in range(1, 7):
                    last = (l == 6)
                    for j in range(G):
                        b.mm(pb[1][:, JS[j]], AT4[:, JS[j]], Tc[:, JS[j]], r=['AT4', kTc], w=[('pb', 1)])
                    b.tt('dve', v3g(N4), v3g(pb[1][:, 0:GWd]), bcm(MA[l]), ALU.mult, r=[('pb', 1), 'cb'], w=['N4'])
                    yield
                    if not last:
                        for j in range(G):
                            b.mm(pb[2][:, JS[j]], Uc[:, JS[j]], N4[:, JS[j]], r=[kUc, 'N4'], w=[('pb', 2)])
                    for j in range(G):
                        b.mm(pb[3][:, JS[j]], N4[:, JS[j]], Uc[:, JS[j]], r=[kUc, 'N4'], w=[('pb', 3)])
                    if not last:
                        b.tt('dve', Tn, Tc, pb[2][:, 0:GWd], ALU.subtract, r=[kTc, ('pb', 2)], w=[kTn_])
                        b.tt('dve', Un, Uc, pb[3][:, 0:GWd], ALU.subtract, r=[kUc, ('pb', 3)], w=[kUn])
                        Tc, kTc, Tn, kTn_ = Tn, kTn_, Tc, kTc
                        Uc, kUc, Un, kUn = Un, kUn, Uc, kUc
                    else:
                        b.tt('dve', U4, Uc, pb[3][:, 0:GWd], ALU.subtract, r=[kUc, ('pb', 3)], w=[kD(2)])
                    yield
                for j in range(G):
                    b.mm(pb[4][:, JS[j]], Kb4[:, JS[j]], U4[:, JS[j]], r=['Kb4', kD(2)], w=[('pb', 4)])
                b.ts('dve', nkc4, pb[4][:, 0:GWd], -1.0, None, ALU.mult, r=[('pb', 4)], w=[kD(3)])
                yield

            def rec(h, d, gi, par):
                hd = d * H + h
                c0 = gi * G
                Kt4 = bv(O_DB[par][0], GWd); Vb4 = bv(O_DB[par][1], GWd); U4 = bv(O_DB[par][2], GWd)
                nkc4 = bv(O_DB[par][3], GWd); attnT4 = bv(O_DB[par][4], GWd); qeT4 = bv(O_DB[par][5], GWd)
                kD = lambda i: ('db', par, i)
                Sf = Sst[:, hd * 128:(hd + 1) * 128]
                Sbf = Sb[:, hd * 128:(hd + 1) * 128]
                order = range(G) if d == 0 else range(G - 1, -1, -1)
                for j in order:
                    c = c0 + j
                    js = slice(j * 128, (j + 1) * 128)
                    cs = slice(c * 128, (c + 1) * 128)
                    vi = vcnt[0] % 2
                    vcnt[0] += 1
                    vnb = vnbs[vi]
                    kv = ('vnb', vi)
                    b.mm(pb[6][:, 0:128], U4[:, js], Vb4[:, js], start=True, stop=False, r=[kD(2), kD(1)], w=[('pb', 6)])
                    b.mm(pb[6][:, 0:128], nkc4[:, js], Sbf, start=False, stop=True, r=[kD(3), ('Sb', hd)], w=[('pb', 6)])
                    b.cp('act', vnb, pb[6][:, 0:128], r=[('pb', 6)], w=[kv])
                    yield
                    if latent:
                        b.mm(pb[5][:, 0:128], qeT4[:, js], Sbf, start=True, stop=False, r=[kD(5), ('Sb', hd)], w=[('pb', 5)])
                        b.mm(pb[5][:, 0:128], attnT4[:, js], vnb, start=False, stop=True, r=[kD(4), kv], w=[('pb', 5)])
                    b.mm(pb[6][:, 128:256], Kt4[:, js], vnb, r=[kD(0), kv], w=[('pb', 6)])
                    b.stt('dve', Sbf, Sf, ecd3[:, c, hd:hd + 1], pb[6][:, 128:256], ALU.mult, ALU.add,
                          r=[('S', hd), 'ecd', ('pb', 6)], w=[('Sb', hd)])
                    b.stt('dve', Sf, Sf, ecd3[:, c, hd:hd + 1], pb[6][:, 128:256], ALU.mult, ALU.add,
                          r=[('S', hd), 'ecd', ('pb', 6)], w=[('S', hd)])
                    if latent:
                        if d == 0:
                            b.cp('act', oacc[:, cs], pb[5][:, 0:128], r=[('pb', 5)], w=[('oacc', c)])
                        else:
                            b.tt('dve', oacc[:, cs], oacc[:, cs], pb[5][:, 0:128], ALU.add, r=[('pb', 5), ('oacc', c)], w=[('oacc', c)])
                    yield

            def run_streams(gens):
                active = list(gens)
                while active:
                    for g_ in list(active):
                        try:
                            next(g_)
                        except StopIteration:
                            active.remove(g_)

            def pre(h):
                bs = sets[h % 2]
                si = h % 2
                srcs = [win_d[:, h * 128:(h + 1) * 128], win_d[:, cfg.V_OFF + h * 128:cfg.V_OFF + (h + 1) * 128]]
                wp1, wk1 = wload(srcs)
                if latent:
                    wp2, wk2 = wload([win_d[:, cfg.Q_OFF + h * 128:cfg.Q_OFF + (h + 1) * 128],
                                      win_d[:, cfg.Z_OFF + h * 128:cfg.Z_OFF + (h + 1) * 128]])
                jobs = [('k', wp1, wk1, 0, h, bs['kTn'], ('kTn', si), 1.0), ('v', wp1, wk1, 128, H + h, bs['vTb'], ('vTb', si), None)]
                if latent:
                    jobs.append(('q', wp2, wk2, 0, 2 * H + h, bs['qTn'], ('qTn', si), 128.0 ** -0.5))
                for ji, job in enumerate(jobs):
                    for j in range(5):
                        b.ts('pool', Dgc[ji * 5 + j], identb, convw[:, job[4] * 5 + j:job[4] * 5 + j + 1], None, ALU.mult,
                             r=['identb', 'convw'], w=[('Dgc', ji * 5 + j)])
                yield

                def silu_gate(i2):
                    t0 = t0s[i2][:, 0:TBk]
                    kt = ('t0', i2)
                    b.act(t0, pb[0][:, 0:TBk], AF.Tanh, scale=0.5, r=[('pb', 0)], w=[kt])
                    b.ts('pool', t0, t0, 0.5, 0.5, ALU.mult, ALU.add, r=[kt], w=[kt])
                    return t0, kt
                for ji, (nm, wp, wk, coff, widx, dst, dkey, scale) in enumerate(jobs):
                    for tb in range(NB):
                        for k in range(KD):
                            b.mm(pb[0][:, 0:TBk], wp[:, k, coff:coff + 128], hsrc[:, k, tb * TBk:(tb + 1) * TBk],
                                 start=(k == 0), stop=(k == KD - 1),
                                 r=[wk] + hkeys[tb * (TBk // 128):(tb + 1) * (TBk // 128)], w=[('pb', 0)])
                        b.cp('act', pad[:, 2 + tb * TBk:2 + (tb + 1) * TBk], pb[0][:, 0:TBk], r=[('pb', 0)], w=['pad'])
                        yield
                    for tb in range(NB):
                        tbs = slice(tb * TBk, (tb + 1) * TBk)
                        i2 = pcnt[0] % 2
                        pcnt[0] += 1
                        for j in range(5):
                            b.mm(pb[0][:, 0:TBk], Dgc[ji * 5 + j], pad[:, j + tb * TBk:j + (tb + 1) * TBk], start=(j == 0),
                                 stop=(j == 4), r=[('Dgc', ji * 5 + j), 'pad'], w=[('pb', 0)])
                        t0, kt = silu_gate(i2)
                        if scale is None:
                            b.tt('dve', dst[:, tbs], pb[0][:, 0:TBk], t0, ALU.mult, r=[('pb', 0), kt], w=[dkey])
                            yield
                            continue
                        cvt = cvs[i2][:, 0:TBk]
                        sqt = sqs[i2][:, 0:TBk]
                        b.tt('dve', cvt, pb[0][:, 0:TBk], t0, ALU.mult, r=[('pb', 0), kt], w=[('cv', i2)])
                        yield
                        b.tt('pool', sqt, cvt, cvt, ALU.mult, r=[('cv', i2)], w=[('sq', i2)])
                        b.mm(pb[0][:, 0:TBk], onesb, sqt, r=[('sq', i2), 'cb'], w=[('pb', 0)])
                        b.act(rr[:, 0:TBk], pb[0][:, 0:TBk], AF.Ln, bias=epst[:, 0:1], r=[('pb', 0), 'eps'], w=['rr'])
                        b.act(rr[:, 0:TBk], rr[:, 0:TBk], AF.Exp, scale=-0.5, r=['rr'], w=['rr'])
                        b.stt('dve', dst[:, tbs], cvt, scale, rr[:, 0:TBk], ALU.mult, ALU.mult, r=[('cv', i2), 'rr'], w=[dkey])
                        yield
                if latent:
                    for tb in range(NB):
                        i2 = pcnt[0] % 2
                        pcnt[0] += 1
                        for k in range(KD):
                            b.mm(pb[0][:, 0:TBk], wp2[:, k, 128:256], hsrc[:, k, tb * TBk:(tb + 1) * TBk],
                                 start=(k == 0), stop=(k == KD - 1),
                                 r=[wk2] + hkeys[tb * (TBk // 128):(tb + 1) * (TBk // 128)], w=[('pb', 0)])
                        t0, kt = silu_gate(i2)
                        b.tt('dve', bs['szT'][:, tb * TBk:(tb + 1) * TBk], pb[0][:, 0:TBk], t0, ALU.mult, r=[('pb', 0), kt],
                             w=[('szT', si)])
                        yield

            def run_main(gens, bg):
                active = list(gens)
                while active:
                    for g_ in list(active):
                        try:
                            next(g_)
                        except StopIteration:
                            active.remove(g_)
                    if bg[0] is not None:
                        try:
                            next(bg[0])
                        except StopIteration:
                            bg[0] = None

            run_streams([pre(0)])
            for h in range(H):
                bg = [pre(h + 1) if h + 1 < H else None]
                szT = sets[h % 2]['szT']
                seq = [(d, gi) for d in range(2) for gi in (range(NGR) if d == 0 else range(NGR - 1, -1, -1))]
                prevR = None
                for idx, (d, gi) in enumerate(seq):
                    gens = [stageA(h, d, gi, idx % 2)]
                    if prevR is not None:
                        gens.append(prevR)
                    run_main(gens, bg)
                    prevR = rec(h, d, gi, idx % 2)
                run_main([prevR], bg)
                if bg[0] is not None:
                    run_streams([bg[0]])
                if latent:
                    oT_all = bv(R_B1, KD * T).rearrange("p (k t) -> p k t", k=KD)
                    for c in range(NCH):
                        cs = slice(c * 128, (c + 1) * 128)
                        rs, rk = rstd_of(oacc[:, cs], [('oacc', c)], 128)
                        on = vnbs[c % 2]
                        k_on = [('vnb', c % 2)]
                        b.stt('dve', on, oacc[:, cs], rs, gdng[:], ALU.mult, ALU.mult, r=[('oacc', c), rk, 'gdng'], w=k_on)
                        b.tr(pT[:, 384:512], on, identb, r=k_on + ['identb'], w=['pT'])
                        b.tt('dve', oT_all[:, h, cs], pT[:, 384:512], szT[:, cs], ALU.mult, r=['pT', ('szT', h % 2)], w=[('oT', h, c)])
            RG.update(base=O_WR, slot=2048, n=3, sbase=O_STG, sslot=2048, pcols=512)
        def phases():
            TB = min(512, T)
            NTB = T // TB
            TPB = TB // 128
            S.barrier()
            if STOP < 1:
                return
            b.memset('pool', Sst, 0.0, w=[('S', i) for i in range(H2)])
            b.memset('pool', Sb, 0.0, w=[('Sb', i) for i in range(H2)])
            hcT = bv(O_HC, KD * TC).rearrange("p (k t) -> p k t", k=KD)
            make_AB(1, 0, 1, 0)
            prenorm(ctx_d, None, NTC, hcT, 'hc')
            S.barrier()
            gdn(hcT, lambda t: ('hc', t), NTC, False, R_B2)
            S.barrier()
            if STOP < 2:
                return
            make_AB(0, 0, 1, 0)
            prenorm(x_d, pos_d, NT, hT, 'hT')
            S.barrier()
            if STOP < 3:
                return
            gdn(hT, lambda t: ('hT', t), NT, True, R_B2)
            S.barrier()
            hkeys_all = [('hT', t) for t in range(NT)]
            oT_all = bv(R_B1, KD * T).rearrange("p (k t) -> p k t", k=KD)
            mergedT = bv(R_B2, KD * T).rearrange("p (k t) -> p k t", k=KD)
            O_T0 = O_X
            tsig = [fv(O_T0 + i * 512, 512) for i in range(3)]
            if STOP < 4:
                return
            for f in range(KD):
                wp, wk = wload([wpa_d[:, f * 128:(f + 1) * 128], win_d[:, cfg.GATE_OFF + f * 128:cfg.GATE_OFF + (f + 1) * 128]])
                for tb in range(NTB):
                    ts_ = slice(tb * TB, (tb + 1) * TB)
                    for k in range(KD):
                        b.mm(pb[0][:, 0:TB], wp[:, k, 0:128], oT_all[:, k, ts_], start=(k == 0), stop=(k == KD - 1),
                             r=[wk, 'oTall'], w=[('pb', 0)])
                    for k in range(KD):
                        b.mm(pb[1][:, 0:TB], wp[:, k, 128:256], hT[:, k, ts_], start=(k == 0), stop=(k == KD - 1),
                             r=[wk] + hkeys_all[tb * TPB:(tb + 1) * TPB], w=[('pb', 1)])
                    sg = tsig[tb % 2]
                    b.act(sg[:, 0:TB], pb[1][:, 0:TB], AF.Sigmoid, r=[('pb', 1)], w=[('tsig', tb % 2)])
                    b.tt('dve', mergedT[:, f, ts_], pb[0][:, 0:TB], sg[:, 0:TB], ALU.mult, r=[('pb', 0), ('tsig', tb % 2)],
                         w=[('mg', f, tb)])
            S.barrier()
            if STOP < 5:
                return
            ucT = bv(R_B1, KD * T).rearrange("p (k t) -> p k t", k=KD)
            O_UP = O_T0 + 3 * 512
            upads = [bv(O_UP, 2 * ((T + 30 + 1) // 2))[:, 0:T + 30] for i in range(2)]
            Dg = [Mt[j // 16][:, (j % 16) * 64:(j % 16) * 64 + 64].bitcast(BF16) for j in range(31)]
            O_UCF = O_UP + (T + 32)
            ucf = fv(O_UCF, T)
            O_S1 = O_UCF + T
            S1 = fv(O_S1, T); S2 = fv(O_S1 + T, T)
            assert O_S1 + 2 * T <= O_WR, (O_S1 + 2 * T, O_WR)
            b.memset('pool', upads[0], 0.0, w=[('up', 0)])
            for ct in range(KD):
                wp, wk = wload([win_d[:, cfg.GLU_OFF + ct * 128:cfg.GLU_OFF + (ct + 1) * 128],
                                win_d[:, cfg.GLU_OFF + D + ct * 128:cfg.GLU_OFF + D + (ct + 1) * 128]])
                up = upads[ct % 2]
                uk = ('up', 0)
                for tb in range(NTB):
                    ts_ = slice(tb * TB, (tb + 1) * TB)
                    for k in range(KD):
                        b.mm(pb[0][:, 0:TB], wp[:, k, 0:128], hT[:, k, ts_], start=(k == 0), stop=(k == KD - 1),
                             r=[wk] + hkeys_all[tb * TPB:(tb + 1) * TPB], w=[('pb', 0)])
                    for k in range(KD):
                        b.mm(pb[1][:, 0:TB], wp[:, k, 128:256], hT[:, k, ts_], start=(k == 0), stop=(k == KD - 1),
                             r=[wk] + hkeys_all[tb * TPB:(tb + 1) * TPB], w=[('pb', 1)])
                    sg = tsig[tb % 2]
                    b.act(sg[:, 0:TB], pb[1][:, 0:TB], AF.Sigmoid, r=[('pb', 1)], w=[('tsig', tb % 2)])
                    b.tt('dve', up[:, 15 + tb * TB:15 + (tb + 1) * TB], pb[0][:, 0:TB], sg[:, 0:TB], ALU.mult,
                         r=[('pb', 0), ('tsig', tb % 2)], w=[uk])
                for j in range(31):
                    b.ts('pool' if j % 2 else 'dve', Dg[j], identb, cdw[:, ct * 31 + j:ct * 31 + j + 1], None, ALU.mult,
                         r=['identb', 'cdw'], w=[('Dg', j)])
                for tb in range(NTB):
                    ts_ = slice(tb * TB, (tb + 1) * TB)
                    for j in range(31):
                        b.mm(pb[4][:, 0:TB], Dg[j], up[:, j + tb * TB:j + (tb + 1) * TB], start=(j == 0), stop=(j == 30),
                             r=[('Dg', j), uk], w=[('pb', 4)])
                    b.act(ucf[:, ts_], pb[4][:, 0:TB], AF.Identity, bias=cdv[:, ct * 3:ct * 3 + 1], r=[('pb', 4), 'cdv'], w=['ucf'])
                b.cp('act', ucT[:, ct, :], ucf, r=['ucf'], w=[('uc', ct)])
                for tb in range(NTB):
                    ts_ = slice(tb * TB, (tb + 1) * TB)
                    sq_ = tsig[2]
                    b.act(sq_[:, 0:TB], ucf[:, ts_], AF.Square, r=['ucf'], w=[('tsig', 2)])
                    b.mm(pb[2][:, 0:TB], onesf, ucf[:, ts_], r=['cf', 'ucf'], w=[('pb', 2)])
                    b.mm(pb[3][:, 0:TB], onesf, sq_[:, 0:TB], r=['cf', ('tsig', 2)], w=[('pb', 3)])
                    if ct == 0:
                        b.cp('act', S1[:, ts_], pb[2][:, 0:TB], r=[('pb', 2)], w=[('S1', tb)])
                        b.cp('act', S2[:, ts_], pb[3][:, 0:TB], r=[('pb', 3)], w=[('S2', tb)])
                    else:
                        b.tt('dve', S1[:, ts_], S1[:, ts_], pb[2][:, 0:TB], ALU.add, r=[('pb', 2), ('S1', tb)], w=[('S1', tb)])
                        b.tt('dve', S2[:, ts_], S2[:, ts_], pb[3][:, 0:TB], ALU.add, r=[('pb', 3), ('S2', tb)], w=[('S2', tb)])
            s1k = [('S1', tb) for tb in range(NTB)]; s2k = [('S2', tb) for tb in range(NTB)]
            b.ts('dve', S1, S1, 1.0 / D, None, ALU.mult, r=s1k, w=s1k)
            b.tt('dve', ucf, S1, S1, ALU.mult, r=s1k + ['ucf'], w=['ucf'])
            b.stt('dve', S2, S2, 1.0 / D, ucf, ALU.mult, ALU.subtract, r=s2k + ['ucf'], w=s2k)
            b.act(S2, S2, AF.Sqrt, bias=epst[:, 0:1], r=s2k + ['eps'], w=s2k)
            b.S.add('dve', lambda e: e.reciprocal(S2, S2), s2k, s2k)
            for ct in range(KD):
                for tb in range(NTB):
                    ts_ = slice(tb * TB, (tb + 1) * TB)
                    t1 = tsig[(ct * NTB + tb) % 2]
                    k1 = ('tsig', (ct * NTB + tb) % 2)
                    b.tt('dve', t1[:, 0:TB], ucT[:, ct, ts_], S1[:, ts_], ALU.subtract, r=[('uc', ct), ('S1', tb)], w=[k1])
                    b.tt('pool', t1[:, 0:TB], t1[:, 0:TB], S2[:, ts_], ALU.mult, r=[k1, ('S2', tb)], w=[k1])
                    b.act(ucT[:, ct, ts_], t1[:, 0:TB], AF.Silu, bias=cdv[:, ct * 3 + 2:ct * 3 + 3], scale=cdv[:, ct * 3 + 1:ct * 3 + 2],
                          r=[k1, 'cdv'], w=[('uc', ct)])
            uck = [('uc', ct) for ct in range(KD)]
            for f in range(KD):
                wp, wk = wload([wpb_d[:, f * 128:(f + 1) * 128],
                                win_d[:, cfg.GATE_OFF + D + f * 128:cfg.GATE_OFF + D + (f + 1) * 128]])
                for tb in range(NTB):
                    ts_ = slice(tb * TB, (tb + 1) * TB)
                    for k in range(KD):
                        b.mm(pb[0][:, 0:TB], wp[:, k, 0:128], ucT[:, k, ts_], start=(k == 0), stop=(k == KD - 1),
                             r=[wk] + uck, w=[('pb', 0)])
                    for k in range(KD):
                        b.mm(pb[1][:, 0:TB], wp[:, k, 128:256], hT[:, k, ts_], start=(k == 0), stop=(k == KD - 1),
                             r=[wk] + hkeys_all[tb * TPB:(tb + 1) * TPB], w=[('pb', 1)])
                    sg = tsig[tb % 2]
                    b.act(sg[:, 0:TB], pb[1][:, 0:TB], AF.Sigmoid, r=[('pb', 1)], w=[('tsig', tb % 2)])
                    b.tt('dve', sg[:, 0:TB], pb[0][:, 0:TB], sg[:, 0:TB], ALU.mult, r=[('pb', 0), ('tsig', tb % 2)],
                         w=[('tsig', tb % 2)])
                    b.tt('pool', mergedT[:, f, ts_], mergedT[:, f, ts_], sg[:, 0:TB], ALU.add,
                         r=[('mg', f, tb), ('tsig', tb % 2)], w=[('mg', f, tb)])
            S.barrier()
            if STOP < 6:
                return
            make_AB(0, 3, 4, 2)
            make_G(0, 2, 1)
            wA, wkA = wload([wout_d[:, 0:256], wout_d[:, 256:512]] if D >= 512 else [wout_d[:, 0:D]])
            if D > 512:
                wB, wkB = wload([wout_d[:, 512:768], wout_d[:, 768:1024]])
            NH = (D + 511) // 512
            HW = min(512, D)
            O5 = R_B1
            yt = fv(O5, D); x1t = [fv(O5 + D + i * D, D) for i in range(2)]; p1t = [fv(O5 + 3 * D + i * D, D) for i in range(2)]
            tmp5 = fv(O5 + 5 * D, D); hb5 = bv(O5 + 6 * D, D)
            wrt = sbt("wrt", [128, KD * E], BF16)
            wrs = fv(O5 + 7 * D, KD * E).rearrange("p (k n) -> p k n", k=KD)
            b.dma('sp', wrs, wr_d.rearrange("(k p) n -> p k n", p=128), w=['wrs'])
            wrt3 = wrt[:].rearrange("p (k n) -> p k n", k=KD)
            b.cp('pool', wrt3, wrs, r=['wrs'], w=['wrt'])
            brt = sbt("brt", [128, E], F32)
            load_bc(brt[:], br_d[0], 'brt')
            lg = sbt("lg", [128, E], F32); mx8 = sbt("mx8", [128, 8], F32); msk = sbt("msk", [128, E], F32)
            sm = sbt("sm", [128, 4], F32)
            mgk = lambda t: [('mg', f, t // TPB) for f in range(KD)]
            for t in range(NT):
                cs = slice(t * 128, (t + 1) * 128)
                for hf in range(NH):
                    wq = wA if hf == 0 else wB
                    for k in range(KD):
                        b.mm(pb[hf][:, 0:HW], mergedT[:, k, cs], wq[:, k, 0:HW], start=(k == 0), stop=(k == KD - 1),
                             r=mgk(t) + [wkA if hf == 0 else wkB], w=[('pb', hf)])
                    b.cp('act', yt[:, hf * HW:(hf + 1) * HW], pb[hf][:, 0:HW], r=[('pb', hf)], w=['yt'])
                rs, rk = rstd_of(yt, ['yt'], D)
                xi = x1t[t % 2]; pi = p1t[t % 2]
                b.dma('sp', xi, x_d[cs, :], w=[('x1', t % 2)])
                b.dma('sp', pi, pos_d[cs, :], w=[('p1', t % 2)])
                b.tt('pool', xi, xi, pi, ALU.add, r=[('x1', t % 2), ('p1', t % 2)], w=[('x1', t % 2)])
                b.stt('dve', tmp5, yt, rs, M[2][:], ALU.mult, ALU.mult, r=['yt', rk, 'M2'], w=['tmp5'])
                b.tt('dve', xi, xi, tmp5, ALU.add, r=[('x1', t % 2), 'tmp5'], w=[('x1', t % 2)])
                b.dma('sp', x2_d[cs, :], xi, r=[('x1', t % 2)], w=[('x2d', t)])
                rs2, rk2 = rstd_of(xi, [('x1', t % 2)], D)
                b.stt('dve', tmp5, xi, rs2, M[0][:], ALU.mult, ALU.mult, r=[('x1', t % 2), rk2, 'M0'], w=['tmp5'])
                b.tt('dve', hb5, tmp5, M[1][:], ALU.add, r=['tmp5', 'M1'], w=['hb5'])
                for k in range(KD):
                    b.tr(pT[:, k * 128:(k + 1) * 128], hb5[:, k * 128:(k + 1) * 128], identb, r=['hb5', 'identb'], w=['pT'])
                b.cp('act', hT[:, :, cs], pT[:, 0:KD * 128].rearrange("p (k n) -> p k n", k=KD), r=['pT'], w=[('hT', t)])
                for k in range(KD):
                    b.mm(pb[2][:, 0:E], hT[:, k, cs], wrt3[:, k, :], start=(k == 0), stop=(k == KD - 1),
                         r=[('hT', t), 'wrt'], w=[('pb', 2)])
                b.tt('dve', lg[:], pb[2][:, 0:E], brt[:], ALU.add, r=[('pb', 2), 'brt'], w=['lg'])
                b.S.add('dve', lambda e: e.max(mx8[:], lg[:]), ['lg'], ['mx8'])
                b.ts('dve', msk[:], lg[:], mx8[:, 3:4], None, ALU.is_ge, r=['lg', 'mx8'], w=['msk'])
                b.ts('dve', sm[:, 0:1], mx8[:, 0:1], -1.0, None, ALU.mult, r=['mx8'], w=['sm'])
                b.act(lg[:], lg[:], AF.Exp, bias=sm[:, 0:1], r=['lg', 'sm'], w=['lg'])
                b.tt('dve', lg[:], lg[:], msk[:], ALU.mult, r=['lg', 'msk'], w=['lg'])
                b.S.add('dve', lambda e: e.reduce_sum(sm[:, 1:2], lg[:], mybir.AxisListType.X), ['lg'], ['sm'])
                b.S.add('dve', lambda e: e.reciprocal(sm[:, 1:2], sm[:, 1:2]), ['sm'], ['sm'])
                b.ts('dve', Wr[:, t * E:(t + 1) * E], lg[:], sm[:, 1:2], None, ALU.mult, r=['lg', 'sm'], w=[('Wr', t)])
            S.barrier()
            if STOP < 7:
                return
            acc = fv(R_B1, NT * D).rearrange("p (t n) -> p t n", t=NT)
            O6 = O_X
            bdt = AR[0:E, O6:O6 + D]
            b.dma('sp', bdt, bd_d, w=['bdt'])
            wrT = AR[0:E, O6 + D:O6 + D + 128]
            for t in range(NT):
                b.tr(pb[3][0:E, 0:128], Wr[:, t * E:(t + 1) * E], identf, r=[('Wr', t), 'cf'], w=[('pb', 3)])
                b.cp('act', wrT, pb[3][0:E, 0:128], r=[('pb', 3)], w=['wrT'])
                for hf in range(NH):
                    b.mm(pb[hf][:, 0:HW], wrT, bdt[:, hf * HW:(hf + 1) * HW], r=['wrT', 'bdt'], w=[('pb', hf)])
                    b.cp('act' if hf == 0 else 'dve', acc[:, t, hf * HW:(hf + 1) * HW], pb[hf][:, 0:HW], r=[('pb', hf)],
                         w=[('acc', t, hf)])
            S.barrier()
            if STOP < 8:
                return
            CASTENG[0] = 'act'
            actT = bv(O_X, KF * T).rearrange("p (k t) -> p k t", k=KF)
            O_MT = O_X + KF * T // 2
            tGs = [fv(O_MT + i * 512, 512) for i in range(2)]
            tSs = [bv(O_MT + 1024 + i * 256, 512) for i in range(2)]
            tLs = [bv(O_MT + 1536 + i * 256, 512) for i in range(2)]
            assert O_MT + 2048 <= O_WR
            GW = min(256, DFF)
            NPJ = DFF // GW
            FPP = GW // 128
            it = 0
            for e in range(E):
                for j in range(NPJ):
                    wp, wk = wload([wgu_d[e][:, j * GW:(j + 1) * GW], wgu_d[e][:, DFF + j * GW:DFF + (j + 1) * GW]])
                    for fi in range(FPP):
                        ft = j * FPP + fi
                        bg = bgu[:, e * 2 * KF + ft:e * 2 * KF + ft + 1]
                        bl = bgu[:, e * 2 * KF + KF + ft:e * 2 * KF + KF + ft + 1]
                        for tb in range(NTB):
                            ts_ = slice(tb * TB, (tb + 1) * TB)
                            pg = it % 2; pl = 2 + it % 2
                            tG = tGs[it % 2]; tS = tSs[it % 2]; tL = tLs[it % 2]
                            kG = ('tG', it % 2); kS = ('tS', it % 2); kL = ('tL', it % 2)
                            it += 1
                            hk = hkeys_all[tb * TPB:(tb + 1) * TPB]
                            for k in range(KD):
                                b.mm(pb[pg][:, 0:TB], wp[:, k, fi * 128:(fi + 1) * 128], hT[:, k, ts_], start=(k == 0),
                                     stop=(k == KD - 1), r=[wk] + hk, w=[('pb', pg)])
                            for k in range(KD):
                                b.mm(pb[pl][:, 0:TB], wp[:, k, GW + fi * 128:GW + (fi + 1) * 128], hT[:, k, ts_], start=(k == 0),
                                     stop=(k == KD - 1), r=[wk] + hk, w=[('pb', pl)])
                            b.ts('dve', tG[:, 0:TB], pb[pg][:, 0:TB], bg, 7.0, ALU.add, ALU.min, r=[('pb', pg), 'bgu'], w=[kG])
                            b.act(tS[:, 0:TB], tG[:, 0:TB], AF.Sigmoid, scale=1.702, r=[kG], w=[kS])
                            b.act(tL[:, 0:TB], pb[pl][:, 0:TB], AF.Identity, bias=bl, r=[('pb', pl), 'bgu'], w=[kL])
                            b.tt('pool', tG[:, 0:TB], tG[:, 0:TB], tS[:, 0:TB], ALU.mult, r=[kG, kS], w=[kG])
                            b.ts('dve', tL[:, 0:TB], tL[:, 0:TB], -7.0, 7.0, ALU.max, ALU.min, r=[kL], w=[kL])
                            b.stt('dve', actT[:, ft, ts_], tL[:, 0:TB], 1.0, tG[:, 0:TB], ALU.add, ALU.mult, r=[kG, kL],
                                  w=[('aT', ft, tb)])
                for hf in range(NH):
                    if HW == 512:
                        srcs = [wd_d[e][:, hf * 512:hf * 512 + 256], wd_d[e][:, hf * 512 + 256:hf * 512 + 512]]
                    else:
                        srcs = [wd_d[e][:, 0:HW]]
                    wp, wk = wload(srcs)
                    for t in range(NT):
                        cs = slice(t * 128, (t + 1) * 128)
                        pd = 4 + t % 2
                        for k in range(KF):
                            b.mm(pb[pd][:, 0:HW], actT[:, k, cs], wp[:, k, 0:HW], start=(k == 0), stop=(k == KF - 1),
                                 r=[wk] + [('aT', k, t // TPB)], w=[('pb', pd)])
                        b.stt('dve', acc[:, t, hf * HW:(hf + 1) * HW], pb[pd][:, 0:HW], Wr[:, t * E + e:t * E + e + 1],
                              acc[:, t, hf * HW:(hf + 1) * HW], ALU.mult, ALU.add, r=[('pb', pd), ('Wr', t), ('acc', t, hf)],
                              w=[('acc', t, hf)])
            S.barrier()

        phases()
        S.barrier()
        acc = fv(R_B1, NT * D).rearrange("p (t n) -> p t n", t=NT)
        NH = (D + 511) // 512
        make_G(0, 5, 3)
        x2t = [fv(O_X + i * D, D) for i in range(2)]
        tmp7 = fv(O_X + 2 * D, D)
        for t in range(NT):
            cs = slice(t * 128, (t + 1) * 128)
            ak = [('acc', t, hf) for hf in range(NH)]
            rs, rk = rstd_of(acc[:, t, :], ak, D)
            xi = x2t[t % 2]
            b.dma('sp', xi, x2_d[cs, :], r=[('x2d', t)], w=[('x2t', t % 2)])
            b.stt('dve', tmp7, acc[:, t, :], rs, M[2][:], ALU.mult, ALU.mult, r=ak + [rk, 'M2'], w=['tmp7'])
            b.tt('dve', xi, xi, tmp7, ALU.add, r=[('x2t', t % 2), 'tmp7'], w=[('x2t', t % 2)])
            b.dma('sp', out_d[cs, :], xi, r=[('x2t', t % 2)], w=[('out', t)])
        S.emit(nc, es)
    return nc


def pos_table(T, D):
    GRID_W = 64
    rows = T // GRID_W
    row = np.repeat(np.arange(rows, dtype=np.float32), GRID_W)
    col = np.tile(np.arange(GRID_W, dtype=np.float32), rows)
    quarter = D // 4
    omega = (np.float32(10000.0) ** (-np.arange(quarter, dtype=np.float32) / np.float32(quarter))).astype(np.float32)

    def emb(p):
        ang = (p[:, None] * omega[None, :]).astype(np.float32)
        return np.concatenate([np.sin(ang), np.cos(ang)], axis=-1)
    return np.concatenate([emb(row), emb(col)], axis=-1).astype(np.float32)


def fm(v, nt):
    return np.ascontiguousarray(np.asarray(v, np.float32).reshape(nt, 128).T)


def host_maps(inp, cfg):
    D, H, T, TC, E, DFF = cfg.D, cfg.H, cfg.T, cfg.TC, cfg.E, cfg.DFF
    KD = D // 128
    KF = DFF // 128
    f = lambda a: np.ascontiguousarray(np.asarray(a, np.float32))
    cf, cb = host_consts()
    pos = pos_table(T, D)
    ckv = f(inp['conv_kv'])[0]
    cq = f(inp['conv_q'])[0]
    convw = np.zeros((128, 3 * H, 5), np.float32)
    for j in range(2 * H):
        convw[:, j, :] = ckv[:, j * 128:(j + 1) * 128].T
    for j in range(H):
        convw[:, 2 * H + j, :] = cq[:, j * 128:(j + 1) * 128].T
    cdwf = f(inp['conf_dw'])[0]
    cdw = np.zeros((128, KD, 31), np.float32)
    for k in range(KD):
        cdw[:, k, :] = cdwf[:, k * 128:(k + 1) * 128].T
    cdv = np.stack([fm(f(inp['conf_dw_b'])[0], KD), fm(f(inp['conf_ln_g'])[0], KD), fm(f(inp['conf_ln_b'])[0], KD)], axis=-1)
    bguf = f(inp['b_gate_up'])[0]
    bgu = np.zeros((128, E, 2 * KF), np.float32)
    for e in range(E):
        bgu[:, e, :] = bguf[e].reshape(2 * KF, 128).T
    shared = {
        "pos": pos, "w_mod": f(inp['w_mod'])[0], "b_mod2": np.ascontiguousarray(np.stack([f(inp['b_mod'])[0]] * 2)),
        "gvecs": np.ascontiguousarray(np.stack([f(inp['g_pre_mix'])[0], f(inp['g_post_mix'])[0], f(inp['g_pre_ffn'])[0], f(inp['g_post_ffn'])[0]])),
        "w_in": f(inp['w_in'])[0], "convw": convw.reshape(128, -1),
        "ab": np.ascontiguousarray(np.stack([f(inp['a_log'])[0].reshape(-1), f(inp['dt_bias'])[0].reshape(-1)])),
        "gdn_g": f(inp['gdn_norm_g'])[0].reshape(1, 128),
        "w_proj_a": f(inp['w_proj_a'])[0], "w_proj_b": f(inp['w_proj_b'])[0], "w_out": f(inp['w_out'])[0],
        "cdw": cdw.reshape(128, -1), "cdv": np.ascontiguousarray(cdv.reshape(128, -1)),
        "w_router": f(inp['w_router'])[0], "b_router": f(inp['b_router'])[0].reshape(1, E),
        "w_gate_up": f(inp['w_gate_up'])[0], "bgu": bgu.reshape(128, -1),
        "w_down": f(inp['w_down'])[0], "b_down": f(inp['b_down'])[0],
        "cf32": cf, "cb16": cb,
    }
    x = f(inp['x']); c = f(inp['c']); ctx = f(inp['ctx']); cctx = f(inp['c_ctx'])
    maps = []
    for bi in range(x.shape[0]):
        cs = np.stack([c[bi], cctx])
        csT = np.ascontiguousarray(cs.reshape(2, KD, 128).transpose(2, 1, 0).reshape(128, KD * 2))
        m = dict(shared)
        m.update({"x": x[bi], "ctx": ctx[bi], "csT": csT})
        maps.append(m)
    return maps


_NC_CACHE = {}


def kernel(**inputs):
    cfg = Cfg()
    if 'nc' not in _NC_CACHE:
        _NC_CACHE['nc'] = build(cfg)
    nc = _NC_CACHE['nc']
    maps = host_maps(inputs, cfg)
    res = run_bass_kernel_spmd(nc, maps, core_ids=list(range(len(maps))))
    return np.stack([np.asarray(r["out"], np.float32) for r in res.results], axis=0)
```

```python
import numpy as np
import ml_dtypes
from contextlib import ExitStack
import concourse.bass as bass
import concourse.mybir as mybir
from concourse.bass_utils import run_bass_kernel_spmd

F32 = mybir.dt.float32
BF16 = mybir.dt.bfloat16
ALU = mybir.AluOpType
AF = mybir.ActivationFunctionType

EPS = 1e-6
BIG = 30000.0
NDMA_SEM = 12


class Sched:
    CE = ('pe', 'act', 'dve', 'pool')

    def __init__(self):
        self.ops = []
        self.lw = {}
        self.rd = {}
        self.last_eng = {}
        self.last_dma = {}
        self.ndq = {}
        self.bdeps = set()

    def barrier(self):
        self.bdeps = set(self.last_eng.values()) | set(self.last_dma.values())

    def add(self, eng, fn, r=(), w=(), dma=False):
        i = len(self.ops)
        psr = [k for k in r if k == 'pT' or (isinstance(k, tuple) and k[0] == 'pb')]
        if psr:
            r = [k for k in r if k not in psr]
            w = list(w) + psr
        deps = set()
        for k in r:
            d = self.lw.get(k)
            if d is not None:
                deps.add(d)
        for k in w:
            d = self.lw.get(k)
            if d is not None:
                deps.add(d)
            for d in self.rd.get(k, ()):
                deps.add(d)
        deps |= self.bdeps
        deps.discard(i)
        if dma:
            n = self.ndq.get(eng, 0)
            self.ndq[eng] = n + 1
            self.last_dma[(eng, n % NDMA_SEM)] = i
        else:
            self.last_eng[eng] = i
        self.ops.append(dict(eng=eng, fn=fn, deps=deps, dma=dma, hasdep=False))
        for k in r:
            self.rd.setdefault(k, []).append(i)
        for k in w:
            self.lw[k] = i
            self.rd[k] = []
        return i

    def plan(self):
        ops = self.ops
        for op in ops:
            for d in op['deps']:
                dop = ops[d]
                if dop['eng'] == 'pe' and op['eng'] == 'pe' and not dop['dma'] and not op['dma']:
                    continue
                dop['hasdep'] = True
        cnt = {e: 0 for e in self.CE}
        ndma = {}
        know = {}
        for op in ops:
            e = op['eng']
            kn = know.setdefault(e, {})
            waits = {}
            for d in sorted(op['deps']):
                dop = ops[d]
                if dop['eng'] == 'pe' and e == 'pe' and not dop['dma'] and not op['dma']:
                    continue
                sem, val = dop['tok']
                if kn.get(sem, 0) >= val:
                    continue
                waits[sem] = max(waits.get(sem, 0), val)
                kn[sem] = val
                for s2, v2 in dop['know'].items():
                    if kn.get(s2, 0) < v2:
                        kn[s2] = v2
            if op['dma']:
                n = ndma.get(e, 0)
                ndma[e] = n + 1
                slot = n % NDMA_SEM
                prev = 16 * (n // NDMA_SEM)
                sem = ('dma', e, slot)
                if prev > 0 and kn.get(sem, 0) < prev:
                    waits[sem] = max(waits.get(sem, 0), prev)
                    kn[sem] = prev
                op['tok'] = (sem, prev + 16)
                op['know'] = {k: v for k, v in kn.items() if k in self.CE}
            else:
                if op['hasdep']:
                    cnt[e] += 1
                    op['tok'] = (e, cnt[e])
                    op['know'] = {k: v for k, v in kn.items() if k in self.CE}
                    op['know'][e] = cnt[e]
                else:
                    op['tok'] = None
            op['waits'] = waits
        self.cnt = cnt
        self.ndma = ndma

    def emit(self, nc, es):
        self.plan()
        sems = {}
        for e in self.CE:
            sems[e] = es.enter_context(nc.semaphore("s_" + e))
        for q in self.ndma:
            for s in range(NDMA_SEM):
                sems[('dma', q, s)] = es.enter_context(nc.semaphore("d_%s_%d" % (q, s)))
        ops = self.ops

        def run(ename, eng):
            for op in ops:
                if op['eng'] != ename:
                    continue
                for sem, val in op['waits'].items():
                    eng.wait_ge(sems[sem], val)
                ins = op['fn'](eng)
                tok = op['tok']
                if tok is not None:
                    ins.then_inc(sems[tok[0]], 16 if op['dma'] else 1)
            if ename in self.ndma:
                n = self.ndma[ename]
                for s in range(min(n, NDMA_SEM)):
                    last = 16 * ((n - 1 - s) // NDMA_SEM + 1)
                    eng.wait_ge(sems[('dma', ename, s)], last)

        with nc.Block() as block:
            @block.tensor
            def _(pe):
                run('pe', pe)

            @block.scalar
            def _(act):
                run('act', act)

            @block.vector
            def _(dve):
                run('dve', dve)

            @block.gpsimd
            def _(pool):
                run('pool', pool)

            @block.sync
            def _(sp):
                run('sp', sp)


class Bld:
    def __init__(self, nc, S):
        self.nc = nc
        self.S = S

    def mm(self, out, lhsT, rhs, start=True, stop=True, r=(), w=()):
        return self.S.add('pe', lambda e: e.matmul(out, lhsT, rhs, start=start, stop=stop), r, w)

    def tr(self, out, in_, ident, r=(), w=()):
        return self.S.add('pe', lambda e: e.transpose(out, in_, ident), r, w)

    def act(self, out, in_, func, bias=None, scale=None, accum_out=None, r=(), w=()):
        kw = {}
        if bias is not None:
            kw['bias'] = bias
        if scale is not None:
            kw['scale'] = scale
        if accum_out is not None:
            kw['accum_out'] = accum_out
        return self.S.add('act', lambda e: e.activation(out, in_, func, **kw), r, w)

    def ts(self, eng, out, in0, s1, s2, op0, op1=None, accum_out=None, r=(), w=()):
        kw = {}
        if op1 is not None:
            kw['op1'] = op1
        if accum_out is not None:
            kw['accum_out'] = accum_out
        return self.S.add(eng, lambda e: e.tensor_scalar(out, in0, s1, s2, op0, **kw), r, w)

    def tt(self, eng, out, in0, in1, op, r=(), w=()):
        return self.S.add(eng, lambda e: e.tensor_tensor(out, in0, in1, op), r, w)

    def stt(self, eng, out, in0, scalar, in1, op0, op1, r=(), w=()):
        return self.S.add(eng, lambda e: e.scalar_tensor_tensor(out, in0, scalar, in1, op0, op1), r, w)

    def cp(self, eng, out, in_, r=(), w=()):
        if eng == 'act':
            return self.S.add('act', lambda e: e.copy(out, in_), r, w)
        return self.S.add(eng, lambda e: e.tensor_copy(out, in_), r, w)

    def memset(self, eng, ap, val, r=(), w=()):
        return self.S.add(eng, lambda e: e.memset(ap, val), r, w)

    def dma(self, q, out, in_, r=(), w=()):
        return self.S.add(q, lambda e: e.dma_start(out, in_), r, w, dma=True)


class Cfg:
    def __init__(s, D=1024, H=8, T=2048, TC=256, E=32, NCORES=8):
        s.D, s.H, s.T, s.TC, s.E, s.NCORES = D, H, T, TC, E, NCORES
        s.DFF = D
        s.V_OFF = H * 128
        s.BETA_OFF = 2 * H * 128
        s.ALPHA_OFF = s.BETA_OFF + 2 * H
        s.STATE = s.ALPHA_OFF + 2 * H
        s.Q_OFF = s.STATE
        s.Z_OFF = s.Q_OFF + H * 128
        s.GLU_OFF = s.Z_OFF + H * 128
        s.GATE_OFF = s.GLU_OFF + 2 * D
        s.NIN = s.GATE_OFF + 2 * D


NCF = 6
NCB = 17


def host_consts():
    i = np.arange(128)
    ident = np.eye(128, dtype=np.float32)
    ones = np.ones((128, 128), np.float32)
    LF = (i[:, None] <= i[None, :]).astype(np.float32)
    LB = (i[:, None] >= i[None, :]).astype(np.float32)
    low_incl = (i[:, None] >= i[None, :])
    PML = np.where(low_incl, 0.0, BIG).astype(np.float32)
    PMU = np.where(low_incl.T, 0.0, BIG).astype(np.float32)
    cf = np.concatenate([ident, ones, LF, LB, PML, PMU], axis=1)
    mls = []
    for l in range(7):
        s = 1 << l
        m = ((i[:, None] // (2 * s) == i[None, :] // (2 * s)) & (i[:, None] % (2 * s) >= s)
             & (i[None, :] % (2 * s) < s)).astype(np.float32)
        mls.append(m)
    cb = [ones] + mls + [m.T for m in mls] + [-mls[0], -mls[0].T]
    cb = np.concatenate(cb, axis=1).astype(ml_dtypes.bfloat16)
    return cf, cb


def build(cfg, STOP=99):
    D, H, T, TC, E, DFF = cfg.D, cfg.H, cfg.T, cfg.TC, cfg.E, cfg.DFF
    KD = D // 128
    KF = DFF // 128
    NT = T // 128
    NTC = TC // 128
    NIN = cfg.NIN
    H2 = 2 * H
    nc = bass.Bass("TRN2", target_bir_lowering=False)

    def din(name, shape, dt=F32):
        return nc.dram_tensor(name, list(shape), dt, kind="ExternalInput").ap()

    x_d = din("x", [T, D]); ctx_d = din("ctx", [TC, D]); pos_d = din("pos", [T, D])
    csT_d = din("csT", [128, KD * 2]); wmod_d = din("w_mod", [D, 6 * D]); bmod_d = din("b_mod2", [2, 6 * D])
    gv_d = din("gvecs", [4, D]); win_d = din("w_in", [D, NIN]); convw_d = din("convw", [128, 3 * H * 5])
    ab_d = din("ab", [2, H2]); gdng_d = din("gdn_g", [1, 128])
    wpa_d = din("w_proj_a", [D, D]); wpb_d = din("w_proj_b", [D, D]); wout_d = din("w_out", [D, D])
    cdw_d = din("cdw", [128, KD * 31]); cdv_d = din("cdv", [128, KD * 3])
    wr_d = din("w_router", [D, E]); br_d = din("b_router", [1, E])
    wgu_d = din("w_gate_up", [E, D, 2 * DFF]); bgu_d = din("bgu", [128, E * 2 * KF])
    wd_d = din("w_down", [E, DFF, D]); bd_d = din("b_down", [E, D])
    cf_d = din("cf32", [128, NCF * 128]); cb_d = din("cb16", [128, NCB * 128], BF16)
    out_d = nc.dram_tensor("out", [T, D], F32, kind="ExternalOutput").ap()
    mod_d = nc.dram_tensor("mod_scr", [2, 6 * D], F32).ap()
    x2_d = nc.dram_tensor("x2_scr", [T, D], F32).ap()

    S = Sched()
    b = Bld(nc, S)
    es = ExitStack()
    with es:
        def sbt(name, shape, dt):
            return es.enter_context(nc.sbuf_tensor("s_" + name, list(shape), dt))

        def pst(name, shape, dt):
            return es.enter_context(nc.psum_tensor(name, list(shape), dt))

        R_HT = 0
        RSZ = max(8192, KD * T // 2)
        R_B1 = RSZ
        R_B2 = 2 * RSZ
        R_SC = 3 * RSZ
        SCW = 20480
        AW = R_SC + SCW
        AR = sbt("arena", [128, AW], F32)
        O_WR = AW - 10240
        O_STG = O_WR + 3 * 2048
        O_X = R_SC
        O_SST = O_WR - 3072

        def fv(off, n):
            return AR[:, off:off + n]

        def bv(off, nbf):
            return AR[:, off:off + nbf // 2].bitcast(BF16)

        hT = bv(R_HT, KD * T).rearrange("p (k t) -> p k t", k=KD)

        cf = sbt("cf", [128, NCF * 128], F32)
        cb = sbt("cb", [128, NCB * 128], BF16)
        b.dma('sp', cf[:], cf_d, w=['cf'])
        b.dma('sp', cb[:], cb_d, w=['cb'])
        identf = cf[:, 0:128]; onesf = cf[:, 128:256]; LFm = cf[:, 256:384]; LBm = cf[:, 384:512]
        PML = cf[:, 512:640]; PMU = cf[:, 640:768]
        identb = identf
        onesb = cb[:, 0:128]
        MLm = [cb[:, (1 + l) * 128:(2 + l) * 128] for l in range(7)]
        MUm = [cb[:, (8 + l) * 128:(9 + l) * 128] for l in range(7)]
        NML0 = cb[:, 15 * 128:16 * 128]; NMU0 = cb[:, 16 * 128:17 * 128]
        identb_t = sbt("identb", [128, 128], BF16)
        b.cp('dve', identb_t[:], identf, r=['cf'], w=['identb'])
        identb = identb_t[:]
        epst = sbt("epst", [128, 1], F32)
        b.memset('dve', epst[:], EPS, w=['eps'])
        stat = sbt("stat", [128, 4 * max(NT, 2) + 8], F32)
        b.memset('dve', stat[:], 0.0, w=['stat'])
        Mt = [sbt("M%d" % i, [128, max(D, 1024)], F32) for i in range(3)]
        M = [Mt[i][:, 0:D] for i in range(3)]
        gtmp = sbt("gtmp", [128, D], F32)
        junk_t = gtmp
        convw = sbt("convw", [128, 3 * H * 5], F32)
        b.dma('sp', convw[:], convw_d, w=['convw'])
        cdw = sbt("cdw", [128, KD * 31], F32); cdv = sbt("cdv", [128, KD * 3], F32)
        b.dma('sp', cdw[:], cdw_d, w=['cdw']); b.dma('sp', cdv[:], cdv_d, w=['cdv'])
        abt = sbt("abt", [128, 2 * H2], F32)
        b.dma('sp', abt[:, 0:H2], ab_d[0].partition_broadcast(128), w=['abt'])
        b.dma('sp', abt[:, H2:2 * H2], ab_d[1].partition_broadcast(128), w=['abt'])
        b.act(abt[:, 0:H2], abt[:, 0:H2], AF.Exp, r=['abt'], w=['abt'])
        b.ts('dve', abt[:, 0:H2], abt[:, 0:H2], -1.0, None, ALU.mult, r=['abt'], w=['abt'])
        gdng = sbt("gdng", [128, 128], F32)
        b.dma('sp', gdng[:], gdng_d[0].partition_broadcast(128), w=['gdng'])
        assert H2 * 128 + H2 * 64 <= 3072
        Sst = fv(O_SST, H2 * 128)
        Sb = bv(O_SST + H2 * 128, H2 * 128)
        bgu = sbt("bgu", [128, E * 2 * KF], F32)
        b.dma('sp', bgu[:], bgu_d, w=['bgu'])
        Wr = sbt("Wr", [128, NT * E], F32)

        pb = [pst("pb%d" % i, [128, 512], F32) for i in range(7)]
        pT = pst("pT", [128, 1024], BF16)

        wr_i = [0]
        NRING = [3]
        RG = dict(base=O_WR, slot=2048, n=3, sbase=O_STG, sslot=2048, pcols=512)
        CASTENG = ['pool']
        stg_i = [0]

        def wload(srcs):
            slot = wr_i[0] % RG['n']
            wr_i[0] += 1
            kc = srcs[0].shape[0] // 128
            piece = bv(RG['base'] + slot * RG['slot'], kc * RG['pcols']).rearrange("p (k n) -> p k n", k=kc)
            c0 = 0
            for src in srcs:
                n = src.shape[1]
                si = stg_i[0] % 2
                stg_i[0] += 1
                st = fv(RG['sbase'] + si * RG['sslot'], kc * n).rearrange("p (k n) -> p k n", k=kc)
                b.dma('sp', st, src.rearrange("(k p) n -> p k n", p=128), w=[('stg', si)])
                b.cp(CASTENG[0], piece[:, :, c0:c0 + n], st, r=[('stg', si)], w=[('wr', slot)])
                c0 += n
            return piece, ('wr', slot)

        def load_bc(dst, row_ap, key, rkeys=()):
            b.dma('sp', dst, row_ap.partition_broadcast(128), r=list(rkeys), w=[key])

        scs = sbt("scs", [128, KD * 2], F32)
        b.dma('sp', scs[:], csT_d, w=['scs'])
        b.act(scs[:], scs[:], AF.Silu, r=['scs'], w=['scs'])
        modsb = AR[0:2, O_X:O_X + 6 * D]
        bmt = AR[0:2, O_X + 6 * D:O_X + 12 * D]
        b.dma('sp', bmt, bmod_d, w=['bmt'])
        for nb in range(6 * D // 512):
            for half in range(2):
                c0 = nb * 512 + half * 256
                si = stg_i[0] % 2
                stg_i[0] += 1
                st = fv(O_STG + si * 2048, KD * 256).rearrange("p (k n) -> p k n", k=KD)
                b.dma('sp', st, wmod_d[:, c0:c0 + 256].rearrange("(k p) n -> p k n", p=128), w=[('stg', si)])
                for k in range(KD):
                    b.mm(pb[0][0:2, half * 256:(half + 1) * 256], scs[:, 2 * k:2 * k + 2], st[:, k, :],
                         start=(k == 0), stop=(k == KD - 1), r=['scs', ('stg', si)], w=[('pb', 0)])
            b.tt('dve', modsb[:, nb * 512:(nb + 1) * 512], pb[0][0:2, :], bmt[:, nb * 512:(nb + 1) * 512], ALU.add,
                 r=[('pb', 0), 'bmt'], w=['modsb'])
        b.dma('sp', mod_d, modsb, r=['modsb'], w=['mod_d'])

        def make_AB(row, jshift, jscale, grow):
            load_bc(M[0][:], mod_d[row, jscale * D:(jscale + 1) * D], 'M0', ['mod_d'])
            load_bc(gtmp[:], gv_d[grow], 'gtmp')
            load_bc(M[1][:], mod_d[row, jshift * D:(jshift + 1) * D], 'M1', ['mod_d'])
            b.stt('dve', M[0][:], M[0][:], 1.0, gtmp[:], ALU.add, ALU.mult, r=['M0', 'gtmp'], w=['M0'])

        def make_G(row, jgate, grow):
            load_bc(M[2][:], mod_d[row, jgate * D:(jgate + 1) * D], 'M2', ['mod_d'])
            load_bc(gtmp[:], gv_d[grow], 'gtmp')
            b.tt('dve', M[2][:], M[2][:], gtmp[:], ALU.mult, r=['M2', 'gtmp'], w=['M2'])

        scol = [0]

        def rstd_of(src_ap, srckeys, width):
            c = scol[0] % (stat.shape[1] // 2)
            scol[0] += 1
            ss = stat[:, 2 * c:2 * c + 1]; rs = stat[:, 2 * c + 1:2 * c + 2]
            junk = junk_t[:, 0:width]
            b.act(junk, src_ap, AF.Square, accum_out=ss, r=list(srckeys) + ['stat'], w=['gtmp', ('st', c)])
            b.act(rs, ss, AF.Sqrt, bias=epst[:, 0:1], scale=1.0 / width, r=[('st', c), 'eps'], w=[('st', c)])
            b.S.add('dve', lambda e: e.reciprocal(rs, rs), [('st', c)], [('st', c)])
            return rs, ('st', c)

        O_XIN = R_B1
        O_PIN = O_XIN + 2 * D
        O_TMP = O_PIN + 2 * D
        O_HB = O_TMP + D
        O_HC = O_HB + D // 2
        O_PN_END = O_HC + KD * TC // 2

        def prenorm(src_d, posd, ntiles, dstT, tagbase):
            for t in range(ntiles):
                xi = fv(O_XIN + (t % 2) * D, D)
                b.dma('sp', xi, src_d[t * 128:(t + 1) * 128, :], w=[('xin', t % 2)])
                if posd is not None:
                    pi = fv(O_PIN + (t % 2) * D, D)
                    b.dma('sp', pi, posd[t * 128:(t + 1) * 128, :], w=[('pin', t % 2)])
                    b.tt('pool', xi, xi, pi, ALU.add, r=[('xin', t % 2), ('pin', t % 2)], w=[('xin', t % 2)])
                rs, rk = rstd_of(xi, [('xin', t % 2)], D)
                tmp = fv(O_TMP, D)
                hb = bv(O_HB, D)
                b.stt('dve', tmp, xi, rs, M[0][:], ALU.mult, ALU.mult, r=[('xin', t % 2), rk, 'M0'], w=['tmp'])
                b.tt('dve', hb, tmp, M[1][:], ALU.add, r=['tmp', 'M1'], w=['hb'])
                for k in range(KD):
                    b.tr(pT[:, k * 128:(k + 1) * 128], hb[:, k * 128:(k + 1) * 128], identb, r=['hb', 'identb'], w=['pT'])
                b.cp('act', dstT[:, :, t * 128:(t + 1) * 128], pT[:, 0:KD * 128].rearrange("p (k n) -> p k n", k=KD),
                     r=['pT'], w=[(tagbase, t)])

        def gdn(hsrc, hkey, NCH, latent, O_GD):
            TT = NCH * 128
            TBk = min(512, TT)
            NB = TT // TBk
            o = [O_GD]

            def alloc(n):
                r = o[0]
                o[0] += n
                return r
            O_PAD = alloc(TT + 4); O_CV = alloc(TT)
            O_KT = alloc(TT // 2); O_QT = alloc(TT // 2); O_VT = alloc(TT // 2); O_ZT = alloc(TT // 2)
            O_SQ = O_PAD + 2
            O_RR = (O_PAD + 2 + TT // 2) if TT >= 1024 else alloc(512)
            O_OACC = alloc(TT)
            NS = NCH * H2
            O_BA = alloc(2 * NS); O_G = alloc(NS); O_BE = alloc(NS); O_GC = alloc(NS)
            O_ETL = alloc(NS); O_BK = alloc(NS); O_TOT = alloc(NS)
            O_EGC = O_BK; O_ECD = O_TOT; O_LNB = O_G; O_NGC = O_BA; O_NGCB = O_BA + NS
            G = min(4, NCH)
            GWd = G * 128
            NGR = NCH // G
            xo = [O_WR + 4096]
            O_SET2 = xo[0]
            xo[0] += 4 * (TT // 2)
            assert xo[0] <= O_WR + 10240

            def abuf():
                if xo[0] + GWd // 2 <= O_WR + 10240:
                    r = xo[0]
                    xo[0] += GWd // 2
                    return r
                return alloc(GWd // 2)
            O_DB = [[abuf() for _ in range(6)] for _ in range(2)]
            O_N4 = abuf(); O_TA = abuf(); O_TB = abuf(); O_UA = abuf(); O_UB = abuf()
            O_VNB = alloc(128)
            assert o[0] <= O_SST, (o[0], O_SST)
            RG.update(base=O_WR, slot=1024, n=2, sbase=O_WR + 2048, sslot=1024, pcols=256)
            hkeys = [hkey(t) for t in range(NCH)]
            pad = fv(O_PAD, TT + 4); cv = fv(O_CV, TT)
            sets = [dict(kTn=bv(O_KT, TT), qTn=bv(O_QT, TT), vTb=bv(O_VT, TT), szT=bv(O_ZT, TT), i=0),
                    dict(kTn=bv(O_SET2, TT), qTn=bv(O_SET2 + TT // 2, TT), vTb=bv(O_SET2 + TT, TT),
                         szT=bv(O_SET2 + 3 * TT // 2, TT), i=1)]
            sq = bv(O_SQ, TT); rr = fv(O_RR, 512); oacc = fv(O_OACC, TT)
            b.memset('pool', pad, 0.0, w=['pad'])
            wp, wk = wload([win_d[:, cfg.BETA_OFF:cfg.BETA_OFF + 2 * H2]])
            ba = fv(O_BA, 2 * NS).rearrange("p (c n) -> p c n", c=NCH)
            for c in range(NCH):
                for k in range(KD):
                    b.mm(pb[1][:, 0:2 * H2], hsrc[:, k, c * 128:(c + 1) * 128], wp[:, k, 0:2 * H2],
                         start=(k == 0), stop=(k == KD - 1), r=[hkeys[c], wk], w=[('pb', 1)])
                b.cp('act', ba[:, c, :], pb[1][:, 0:2 * H2], r=[('pb', 1)], w=['ba'])
            v3 = lambda off: fv(off, NS).rearrange("p (c n) -> p c n", c=NCH)
            g3 = v3(O_G); be3 = v3(O_BE); gc3 = v3(O_GC); egc3 = v3(O_EGC); etl3 = v3(O_ETL); ecd3 = v3(O_ECD)
            bk3 = v3(O_BK); tot3 = v3(O_TOT)
            b.act(be3, ba[:, :, 0:H2], AF.Sigmoid, r=['ba'], w=['be'])
            dtb = abt[:, H2:2 * H2]
            nA = abt[:, 0:H2]
            for c in range(NCH):
                b.tt('dve', g3[:, c, :], ba[:, c, H2:2 * H2], dtb, ALU.add, r=['ba', 'abt'], w=['g'])
            b.act(fv(O_G, NS), fv(O_G, NS), AF.Exp, r=['g'], w=['g'])
            b.act(fv(O_G, NS), fv(O_G, NS), AF.Ln, bias=1.0, r=['g'], w=['g'])
            for c in range(NCH):
                b.tt('dve', g3[:, c, :], g3[:, c, :], nA, ALU.mult, r=['g', 'abt'], w=['g'])
            b.mm(pb[1][:, 0:NS], LFm, fv(O_G, NS), r=['g', 'cf'], w=[('pb', 1)])
            b.cp('dve', gc3[:, :, 0:H], pb[1][:, 0:NS].rearrange("p (c n) -> p c n", c=NCH)[:, :, 0:H], r=[('pb', 1)], w=['gc'])
            b.mm(pb[1][:, 0:NS], LBm, fv(O_G, NS), r=['g', 'cf'], w=[('pb', 1)])
            b.cp('dve', gc3[:, :, H:H2], pb[1][:, 0:NS].rearrange("p (c n) -> p c n", c=NCH)[:, :, H:H2], r=[('pb', 1)], w=['gc'])
            b.mm(pb[1][:, 0:NS], onesf, fv(O_G, NS), r=['g', 'cf'], w=[('pb', 1)])
            b.cp('dve', fv(O_TOT, NS), pb[1][:, 0:NS], r=[('pb', 1)], w=['tot'])
            b.act(fv(O_BK, NS), fv(O_GC, NS), AF.Exp, r=['gc'], w=['bk'])
            b.tt('dve', fv(O_BK, NS), fv(O_BE, NS), fv(O_BK, NS), ALU.mult, r=['be', 'bk'], w=['bk'])
            b.tt('dve', fv(O_ETL, NS), fv(O_TOT, NS), fv(O_GC, NS), ALU.subtract, r=['tot', 'gc'], w=['etl'])
            b.act(fv(O_ETL, NS), fv(O_ETL, NS), AF.Exp, r=['etl'], w=['etl'])
            b.act(fv(O_TOT, NS), fv(O_TOT, NS), AF.Exp, r=['tot', 'etl'], w=['tot', 'ecd'])
            b.act(fv(O_LNB, NS), fv(O_BE, NS), AF.Ln, r=['be', 'g', ('pb', 1)], w=['g', 'lnb'])
            b.ts('dve', fv(O_NGC, NS), fv(O_GC, NS), -1.0, None, ALU.mult, r=['gc', 'ba', 'be'], w=['ba', 'ngc'])
            b.stt('dve', fv(O_NGCB, NS), fv(O_LNB, NS), -1.0, fv(O_NGC, NS), ALU.mult, ALU.add, r=['lnb', 'ngc'], w=['ba', 'ngcb'])
            ngc3 = v3(O_NGC); ngcb3 = v3(O_NGCB)
            sckeys = ['gc', 'egc', 'ecd', 'etl', 'bk', 'be']

            tb_ = [0]

            def tmpa(nwords):
                n = (nwords + 63) // 64 * 64
                if tb_[0] % 1024 + n > 1024:
                    tb_[0] = (tb_[0] // 1024 + 1) * 1024
                r = O_TMPS + tb_[0] % 1024
                tb_[0] += n
                return r, [('tmw', (r - O_TMPS) // 64 + i) for i in range(n // 64)]

            fixc = [0]

            def fixa(i):
                par = fixc[0] % 2
                return O_FIX + (par * 9 + i) * 64, [('fix', par, i)]

            def conv5(widx, src_w, func_after):
                eng = 'dve'
                for j in range(5):
                    wcol = convw[:, widx * 5 + j:widx * 5 + j + 1]
                    if j == 0:
                        b.ts(eng, cv, pad[:, 0:TT], wcol, None, ALU.mult, r=['pad', 'convw'], w=['cv'])
                    else:
                        b.stt(eng, cv, pad[:, j:j + TT], wcol, cv, ALU.mult, ALU.add, r=['pad', 'convw', 'cv'], w=['cv'])
                b.act(cv, cv, AF.Silu, r=['cv'], w=['cv'])

            def proj_to_pad(wp, wk, coff):
                for tb in range(NB):
                    for k in range(KD):
                        b.mm(pb[0][:, 0:TBk], wp[:, k, coff:coff + 128], hsrc[:, k, tb * TBk:(tb + 1) * TBk],
                             start=(k == 0), stop=(k == KD - 1),
                             r=[wk] + hkeys[tb * (TBk // 128):(tb + 1) * (TBk // 128)], w=[('pb', 0)])
                    b.cp('act', pad[:, 2 + tb * TBk:2 + (tb + 1) * TBk], pb[0][:, 0:TBk], r=[('pb', 0)], w=['pad'])

            def l2n(dst, scale):
                b.act(sq, cv, AF.Square, r=['cv'], w=['pad'])
                for tb in range(NB):
                    b.mm(pb[1][:, 0:TBk], onesb, sq[:, tb * TBk:(tb + 1) * TBk], r=['pad', 'cb'], w=[('pb', 1)])
                    b.act(rr[:, 0:TBk], pb[1][:, 0:TBk], AF.Sqrt, bias=epst[:, 0:1], r=[('pb', 1), 'eps'], w=['pad'])
                    b.S.add('dve', lambda e, TBk=TBk: e.reciprocal(rr[:, 0:TBk], rr[:, 0:TBk]), ['pad'], ['pad'])
                    b.stt('dve', dst[:, tb * TBk:(tb + 1) * TBk], cv[:, tb * TBk:(tb + 1) * TBk], scale, rr[:, 0:TBk],
                          ALU.mult, ALU.mult, r=['cv', 'pad'], w=[dst_key[0]])

            v3g = lambda ap: ap.rearrange("p (g n) -> p g n", g=G)
            bcm = lambda m: m.unsqueeze(1).to_broadcast([128, G, 128])
            dg4 = Mt[0][:, 0:GWd]; ndg4 = Mt[0][:, 512:512 + GWd]; ndgb4 = Mt[1][:, 0:GWd]
            m1b = Mt[1][:, 512:1024].bitcast(BF16)
            E4 = m1b[:, 0:GWd]; E24 = m1b[:, 512:512 + GWd]
            m2b = Mt[2][:, 0:1024].bitcast(BF16)
            Eg4 = m2b[:, 0:GWd]; Kb4 = m2b[:, 512:512 + GWd]; A4 = m2b[:, 1024:1024 + GWd]; AT4 = m2b[:, 1536:1536 + GWd]
            N4 = bv(O_N4, GWd); Ta = bv(O_TA, GWd); Tb_ = bv(O_TB, GWd); Ua = bv(O_UA, GWd); Ub = bv(O_UB, GWd)
            vnbs = [bv(O_VNB + i * 64, 128) for i in range(2)]
            vcnt = [0]

            def stageA(h, d, gi, par):
                bs = sets[h % 2]
                kTn = bs['kTn']; qTn = bs['qTn']; vTb = bs['vTb']
                kK = ('kTn', h % 2); kQ = ('qTn', h % 2); kV = ('vTb', h % 2)
                hd = d * H + h
                c0 = gi * G
                PMA = PML if d == 0 else PMU
                PMT = PMU if d == 0 else PML
                MA = MLm if d == 0 else MUm
                NMA0 = NML0 if d == 0 else NMU0
                NMT0 = NMU0 if d == 0 else NML0
                csl = slice(c0 * 128, (c0 + G) * 128)
                bcs = lambda t3: t3[:, c0:c0 + G, hd:hd + 1].to_broadcast([128, G, 128])
                Kt4 = bv(O_DB[par][0], GWd); Vb4 = bv(O_DB[par][1], GWd); U4 = bv(O_DB[par][2], GWd)
                nkc4 = bv(O_DB[par][3], GWd); attnT4 = bv(O_DB[par][4], GWd); qeT4 = bv(O_DB[par][5], GWd)
                kD = lambda i: ('db', par, i)
                JS = [slice(j * 128, (j + 1) * 128) for j in range(G)]
                CS = [slice((c0 + j) * 128, (c0 + j + 1) * 128) for j in range(G)]
                for j in range(G):
                    b.tr(pT[:, JS[j]], kTn[:, CS[j]], identb, r=[kK, 'identb'], w=['pT'])
                    b.tr(pT[:, 512 + j * 128:512 + (j + 1) * 128], vTb[:, CS[j]], identb, r=[kV, 'identb'], w=['pT'])
                b.tt('dve', v3g(Kb4), v3g(pT[:, 0:GWd]), bcs(bk3), ALU.mult, r=['pT', 'bk'], w=['Kb4'])
                b.tt('dve', v3g(Kt4), v3g(pT[:, 0:GWd]), bcs(etl3), ALU.mult, r=['pT', 'etl'], w=[kD(0)])
                b.tt('dve', v3g(Vb4), v3g(pT[:, 512:512 + GWd]), bcs(be3), ALU.mult, r=['pT', 'be'], w=[kD(1)])
                yield
                b.tt('pool', v3g(dg4), bcm(identf), bcs(gc3), ALU.mult, r=['cf', 'gc'], w=['dg4'])
                b.tt('pool', v3g(ndgb4), bcm(identf), bcs(ngcb3), ALU.mult, r=['cf', 'ngcb'], w=['ndgb4'])
                for j in range(G):
                    b.mm(pb[1][:, JS[j]], onesf, dg4[:, JS[j]], start=True, stop=False, r=['cf', 'dg4'], w=[('pb', 1)])
                    b.mm(pb[1][:, JS[j]], ndgb4[:, JS[j]], onesf, start=False, stop=False, r=['cf', 'ndgb4'], w=[('pb', 1)])
                    b.mm(pb[1][:, JS[j]], identf, PMA, start=False, stop=True, r=['cf'], w=[('pb', 1)])
                b.act(E4, pb[1][:, 0:GWd], AF.Exp, scale=-1.0, r=[('pb', 1)], w=['E4'])
                for j in range(G):
                    b.mm(pb[4][:, JS[j]], kTn[:, CS[j]], kTn[:, CS[j]], r=[kK], w=[('pb', 4)])
                b.tt('dve', A4, pb[4][:, 0:GWd], E4, ALU.mult, r=[('pb', 4), 'E4'], w=['A4'])
                if latent:
                    b.tt('pool', v3g(ndg4), bcm(identf), bcs(ngc3), ALU.mult, r=['cf', 'ngc'], w=['ndg4'])
                    for j in range(G):
                        b.mm(pb[2][:, JS[j]], onesf, ndg4[:, JS[j]], start=True, stop=False, r=['cf', 'ndg4'], w=[('pb', 2)])
                        b.mm(pb[2][:, JS[j]], dg4[:, JS[j]], onesf, start=False, stop=False, r=['cf', 'dg4'], w=[('pb', 2)])
                        b.mm(pb[2][:, JS[j]], identf, PMT, start=False, stop=True, r=['cf'], w=[('pb', 2)])
                    b.act(E24, pb[2][:, 0:GWd], AF.Exp, scale=-1.0, r=[('pb', 2)], w=['E24'])
                    for j in range(G):
                        b.mm(pb[3][:, JS[j]], onesf, dg4[:, JS[j]], r=['cf', 'dg4'], w=[('pb', 3)])
                    b.act(Eg4, pb[3][:, 0:GWd], AF.Exp, r=[('pb', 3)], w=['Eg4'])
                    for j in range(G):
                        b.mm(pb[5][:, JS[j]], kTn[:, CS[j]], qTn[:, CS[j]], r=[kK, kQ], w=[('pb', 5)])
                    b.tt('dve', attnT4, pb[5][:, 0:GWd], E24, ALU.mult, r=[('pb', 5), 'E24'], w=[kD(4)])
                    b.tt('pool', qeT4, qTn[:, csl], Eg4, ALU.mult, r=[kQ, 'Eg4'], w=[kD(5)])
                yield
                for j in range(G):
                    b.tr(pT[:, JS[j]], A4[:, JS[j]], identb, r=['A4', 'identb'], w=['pT'])
                b.cp('act', AT4, pT[:, 0:GWd], r=['pT'], w=['AT4'])
                b.tt('pool', v3g(Ta), v3g(A4), bcm(NMA0), ALU.mult, r=['A4', 'cb'], w=['Ta'])
                b.tt('pool', v3g(Ta), v3g(Ta), bcm(identb), ALU.add, r=['Ta', 'identb'], w=['Ta'])
                b.tt('pool', v3g(Ua), v3g(AT4), bcm(NMT0), ALU.mult, r=['AT4', 'cb'], w=['Ua'])
                b.tt('pool', v3g(Ua), v3g(Ua), bcm(identb), ALU.add, r=['Ua', 'identb'], w=['Ua'])
                yield
                Tc, kTc, Tn, kTn_ = Ta, 'Ta', Tb_, 'Tb'
                Uc, kUc, Un, kUn = Ua, 'Ua', Ub, 'Ub'
                for l in range(1, 7):
                    last = (l == 6)
                    for j in range(G):
                        b.mm(pb[1][:, JS[j]], AT4[:, JS[j]], Tc[:, JS[j]], r=['AT4', kTc], w=[('pb', 1)])
                    b.tt('dve', v3g(N4), v3g(pb[1][:, 0:GWd]), bcm(MA[l]), ALU.mult, r=[('pb', 1), 'cb'], w=['N4'])
                    yield
                    if not last:
                        for j in range(G):
                            b.mm(pb[2][:, JS[j]], Uc[:, JS[j]], N4[:, JS[j]], r=[kUc, 'N4'], w=[('pb', 2)])
                    for j in range(G):
                        b.mm(pb[3][:, JS[j]], N4[:, JS[j]], Uc[:, JS[j]], r=[kUc, 'N4'], w=[('pb', 3)])
                    if not last:
                        b.tt('dve', Tn, Tc, pb[2][:, 0:GWd], ALU.subtract, r=[kTc, ('pb', 2)], w=[kTn_])
                        b.tt('dve', Un, Uc, pb[3][:, 0:GWd], ALU.subtract, r=[kUc, ('pb', 3)], w=[kUn])
                        Tc, kTc, Tn, kTn_ = Tn, kTn_, Tc, kTc
                        Uc, kUc, Un, kUn = Un, kUn, Uc, kUc
                    else:
                        b.tt('dve', U4, Uc, pb[3][:, 0:GWd], ALU.subtract, r=[kUc, ('pb', 3)], w=[kD(2)])
                    yield
                for j in range(G):
                    b.mm(pb[4][:, JS[j]], Kb4[:, JS[j]], U4[:, JS[j]], r=['Kb4', kD(2)], w=[('pb', 4)])
                b.ts('dve', nkc4, pb[4][:, 0:GWd], -1.0, None, ALU.mult, r=[('pb', 4)], w=[kD(3)])
                yield

            def rec(h, d, gi, par):
                hd = d * H + h
                c0 = gi * G
                Kt4 = bv(O_DB[par][0], GWd); Vb4 = bv(O_DB[par][1], GWd); U4 = bv(O_DB[par][2], GWd)
                nkc4 = bv(O_DB[par][3], GWd); attnT4 = bv(O_DB[par][4], GWd); qeT4 = bv(O_DB[par][5], GWd)
                kD = lambda i: ('db', par, i)
                Sf = Sst[:, hd * 128:(hd + 1) * 128]
                Sbf = Sb[:, hd * 128:(hd + 1) * 128]
                order = range(G) if d == 0 else range(G - 1, -1, -1)
                for j in order:
                    c = c0 + j
                    js = slice(j * 128, (j + 1) * 128)
                    cs = slice(c * 128, (c + 1) * 128)
                    vi = vcnt[0] % 2
                    vcnt[0] += 1
                    vnb = vnbs[vi]
                    kv = ('vnb', vi)
                    b.mm(pb[6][:, 0:128], U4[:, js], Vb4[:, js], start=True, stop=False, r=[kD(2), kD(1)], w=[('pb', 6)])
                    b.mm(pb[6][:, 0:128], nkc4[:, js], Sbf, start=False, stop=True, r=[kD(3), ('Sb', hd)], w=[('pb', 6)])
                    b.cp('act', vnb, pb[6][:, 0:128], r=[('pb', 6)], w=[kv])
                    yield
                    if latent:
                        b.mm(pb[5][:, 0:128], qeT4[:, js], Sbf, start=True, stop=False, r=[kD(5), ('Sb', hd)], w=[('pb', 5)])
                        b.mm(pb[5][:, 0:128], attnT4[:, js], vnb, start=False, stop=True, r=[kD(4), kv], w=[('pb', 5)])
                    b.mm(pb[6][:, 128:256], Kt4[:, js], vnb, r=[kD(0), kv], w=[('pb', 6)])
                    b.stt('dve', Sf, Sf, ecd3[:, c, hd:hd + 1], pb[6][:, 128:256], ALU.mult, ALU.add,
                          r=[('S', hd), 'ecd', ('pb', 6)], w=[('S', hd)])
                    b.cp('act', Sbf, Sf, r=[('S', hd)], w=[('Sb', hd)])
                    if latent:
                        if d == 0:
                            b.cp('act', oacc[:, cs], pb[5][:, 0:128], r=[('pb', 5)], w=[('oacc', c)])
                        else:
                            b.tt('dve', oacc[:, cs], oacc[:, cs], pb[5][:, 0:128], ALU.add, r=[('pb', 5), ('oacc', c)], w=[('oacc', c)])
                    yield

            def run_streams(gens):
                active = list(gens)
                while active:
                    for g_ in list(active):
                        try:
                            next(g_)
                        except StopIteration:
                            active.remove(g_)

            def pre(h):
                bs = sets[h % 2]
                si = h % 2
                srcs = [win_d[:, h * 128:(h + 1) * 128], win_d[:, cfg.V_OFF + h * 128:cfg.V_OFF + (h + 1) * 128]]
                wp1, wk1 = wload(srcs)
                if latent:
                    wp2, wk2 = wload([win_d[:, cfg.Q_OFF + h * 128:cfg.Q_OFF + (h + 1) * 128],
                                      win_d[:, cfg.Z_OFF + h * 128:cfg.Z_OFF + (h + 1) * 128]])
                yield
                jobs = [('k', wp1, wk1, 0, h, bs['kTn'], ('kTn', si), 1.0), ('v', wp1, wk1, 128, H + h, bs['vTb'], ('vTb', si), None)]
                if latent:
                    jobs.append(('q', wp2, wk2, 0, 2 * H + h, bs['qTn'], ('qTn', si), 128.0 ** -0.5))
                for (nm, wp, wk, coff, widx, dst, dkey, scale) in jobs:
                    for tb in range(NB):
                        for k in range(KD):
                            b.mm(pb[0][:, 0:TBk], wp[:, k, coff:coff + 128], hsrc[:, k, tb * TBk:(tb + 1) * TBk],
                                 start=(k == 0), stop=(k == KD - 1),
                                 r=[wk] + hkeys[tb * (TBk // 128):(tb + 1) * (TBk // 128)], w=[('pb', 0)])
                        b.cp('act', pad[:, 2 + tb * TBk:2 + (tb + 1) * TBk], pb[0][:, 0:TBk], r=[('pb', 0)], w=['pad'])
                        yield
                    for j in range(5):
                        wcol = convw[:, widx * 5 + j:widx * 5 + j + 1]
                        if j == 0:
                            b.ts('dve', cv, pad[:, 0:TT], wcol, None, ALU.mult, r=['pad', 'convw'], w=['cv'])
                        else:
                            b.stt('dve', cv, pad[:, j:j + TT], wcol, cv, ALU.mult, ALU.add, r=['pad', 'convw', 'cv'], w=['cv'])
                        yield
                    b.act(cv, cv, AF.Silu, r=['cv'], w=['cv'])
                    if scale is None:
                        b.cp('pool', dst, cv, r=['cv'], w=[dkey])
                        yield
                        continue
                    b.act(sq, cv, AF.Square, r=['cv'], w=['pad'])
                    yield
                    for tb in range(NB):
                        b.mm(pb[0][:, 0:TBk], onesb, sq[:, tb * TBk:(tb + 1) * TBk], r=['pad', 'cb'], w=[('pb', 0)])
                        b.act(rr[:, 0:TBk], pb[0][:, 0:TBk], AF.Sqrt, bias=epst[:, 0:1], r=[('pb', 0), 'eps'], w=['pad'])
                        b.S.add('dve', lambda e, TBk=TBk: e.reciprocal(rr[:, 0:TBk], rr[:, 0:TBk]), ['pad'], ['pad'])
                        b.stt('dve', dst[:, tb * TBk:(tb + 1) * TBk], cv[:, tb * TBk:(tb + 1) * TBk], scale, rr[:, 0:TBk],
                              ALU.mult, ALU.mult, r=['cv', 'pad'], w=[dkey])
                        yield
                if latent:
                    for tb in range(NB):
                        for k in range(KD):
                            b.mm(pb[0][:, 0:TBk], wp2[:, k, 128:256], hsrc[:, k, tb * TBk:(tb + 1) * TBk],
                                 start=(k == 0), stop=(k == KD - 1),
                                 r=[wk2] + hkeys[tb * (TBk // 128):(tb + 1) * (TBk // 128)], w=[('pb', 0)])
                        b.act(bs['szT'][:, tb * TBk:(tb + 1) * TBk], pb[0][:, 0:TBk], AF.Silu, r=[('pb', 0)], w=[('szT', si)])
                        yield

            def run_main(gens, bg):
                active = list(gens)
                while active:
                    for g_ in list(active):
                        try:
                            next(g_)
                        except StopIteration:
                            active.remove(g_)
                    if bg[0] is not None:
                        try:
                            next(bg[0])
                        except StopIteration:
                            bg[0] = None

            def emit_onorm(h):
                szT = sets[h % 2]['szT']
                oT_all = bv(R_B1, KD * T).rearrange("p (k t) -> p k t", k=KD)
                for c in range(NCH):
                    cs = slice(c * 128, (c + 1) * 128)
                    rs, rk = rstd_of(oacc[:, cs], [('oacc', c)], 128)
                    on = vnbs[c % 2]
                    k_on = [('vnb', c % 2)]
                    b.stt('dve', on, oacc[:, cs], rs, gdng[:], ALU.mult, ALU.mult, r=[('oacc', c), rk, 'gdng'], w=k_on)
                    b.tr(pT[:, 384:512], on, identb, r=k_on + ['identb'], w=['pT'])
                    b.tt('dve', oT_all[:, h, cs], pT[:, 384:512], szT[:, cs], ALU.mult, r=['pT', ('szT', h % 2)], w=[('oT', h, c)])

            run_streams([pre(0)])
            prevR = None
            pend = None
            gidx = 0
            for h in range(H):
                bg = [pre(h + 1) if h + 1 < H else None]
                seq = [(d, gi) for d in range(2) for gi in (range(NGR) if d == 0 else range(NGR - 1, -1, -1))]
                for i, (d, gi) in enumerate(seq):
                    gens = [stageA(h, d, gi, gidx % 2)]
                    if prevR is not None:
                        gens.append(prevR)
                    if i == 0:
                        run_main(gens, [None])
                        if pend is not None:
                            emit_onorm(pend)
                            pend = None
                    else:
                        run_main(gens, bg)
                    prevR = rec(h, d, gi, gidx % 2)
                    gidx += 1
                if bg[0] is not None:
                    run_streams([bg[0]])
                if latent:
                    pend = h
            run_streams([prevR])
            if pend is not None:
                emit_onorm(pend)
            RG.update(base=O_WR, slot=2048, n=3, sbase=O_STG, sslot=2048, pcols=512)
        def phases():
            TB = min(512, T)
            NTB = T // TB
            TPB = TB // 128
            S.barrier()
            if STOP < 1:
                return
            b.memset('pool', Sst, 0.0, w=[('S', i) for i in range(H2)])
            b.memset('pool', Sb, 0.0, w=[('Sb', i) for i in range(H2)])
            hcT = bv(O_HC, KD * TC).rearrange("p (k t) -> p k t", k=KD)
            make_AB(1, 0, 1, 0)
            prenorm(ctx_d, None, NTC, hcT, 'hc')
            gdn(hcT, lambda t: ('hc', t), NTC, False, R_B2)
            S.barrier()
            if STOP < 2:
                return
            make_AB(0, 0, 1, 0)
            prenorm(x_d, pos_d, NT, hT, 'hT')
            S.barrier()
            if STOP < 3:
                return
            gdn(hT, lambda t: ('hT', t), NT, True, R_B2)
            S.barrier()
            hkeys_all = [('hT', t) for t in range(NT)]
            oT_all = bv(R_B1, KD * T).rearrange("p (k t) -> p k t", k=KD)
            mergedT = bv(R_B2, KD * T).rearrange("p (k t) -> p k t", k=KD)
            O_T0 = O_X
            tsig = [fv(O_T0 + i * 512, 512) for i in range(3)]
            if STOP < 4:
                return
            for f in range(KD):
                wp, wk = wload([wpa_d[:, f * 128:(f + 1) * 128], win_d[:, cfg.GATE_OFF + f * 128:cfg.GATE_OFF + (f + 1) * 128]])
                for tb in range(NTB):
                    ts_ = slice(tb * TB, (tb + 1) * TB)
                    for k in range(KD):
                        b.mm(pb[0][:, 0:TB], wp[:, k, 0:128], oT_all[:, k, ts_], start=(k == 0), stop=(k == KD - 1),
                             r=[wk, 'oTall'], w=[('pb', 0)])
                    for k in range(KD):
                        b.mm(pb[1][:, 0:TB], wp[:, k, 128:256], hT[:, k, ts_], start=(k == 0), stop=(k == KD - 1),
                             r=[wk] + hkeys_all[tb * TPB:(tb + 1) * TPB], w=[('pb', 1)])
                    sg = tsig[tb % 2]
                    b.act(sg[:, 0:TB], pb[1][:, 0:TB], AF.Sigmoid, r=[('pb', 1)], w=[('tsig', tb % 2)])
                    b.tt('dve', mergedT[:, f, ts_], pb[0][:, 0:TB], sg[:, 0:TB], ALU.mult, r=[('pb', 0), ('tsig', tb % 2)],
                         w=[('mg', f, tb)])
            S.barrier()
            if STOP < 5:
                return
            ucT = bv(R_B1, KD * T).rearrange("p (k t) -> p k t", k=KD)
            O_UP = O_T0 + 3 * 512
            upads = [bv(O_UP, 2 * ((T + 30 + 1) // 2))[:, 0:T + 30] for i in range(2)]
            Dg = [Mt[j // 16][:, (j % 16) * 64:(j % 16) * 64 + 64].bitcast(BF16) for j in range(31)]
            O_UCF = O_UP + (T + 32)
            ucf = fv(O_UCF, T)
            O_S1 = O_UCF + T
            S1 = fv(O_S1, T); S2 = fv(O_S1 + T, T)
            assert O_S1 + 2 * T <= O_WR, (O_S1 + 2 * T, O_WR)
            b.memset('pool', upads[0], 0.0, w=[('up', 0)])
            for ct in range(KD):
                wp, wk = wload([win_d[:, cfg.GLU_OFF + ct * 128:cfg.GLU_OFF + (ct + 1) * 128],
                                win_d[:, cfg.GLU_OFF + D + ct * 128:cfg.GLU_OFF + D + (ct + 1) * 128]])
                up = upads[ct % 2]
                uk = ('up', 0)
                for tb in range(NTB):
                    ts_ = slice(tb * TB, (tb + 1) * TB)
                    for k in range(KD):
                        b.mm(pb[0][:, 0:TB], wp[:, k, 0:128], hT[:, k, ts_], start=(k == 0), stop=(k == KD - 1),
                             r=[wk] + hkeys_all[tb * TPB:(tb + 1) * TPB], w=[('pb', 0)])
                    for k in range(KD):
                        b.mm(pb[1][:, 0:TB], wp[:, k, 128:256], hT[:, k, ts_], start=(k == 0), stop=(k == KD - 1),
                             r=[wk] + hkeys_all[tb * TPB:(tb + 1) * TPB], w=[('pb', 1)])
                    sg = tsig[tb % 2]
                    b.act(sg[:, 0:TB], pb[1][:, 0:TB], AF.Sigmoid, r=[('pb', 1)], w=[('tsig', tb % 2)])
                    b.tt('dve', up[:, 15 + tb * TB:15 + (tb + 1) * TB], pb[0][:, 0:TB], sg[:, 0:TB], ALU.mult,
                         r=[('pb', 0), ('tsig', tb % 2)], w=[uk])
                for j in range(31):
                    b.ts('pool' if j % 2 else 'dve', Dg[j], identb, cdw[:, ct * 31 + j:ct * 31 + j + 1], None, ALU.mult,
                         r=['identb', 'cdw'], w=[('Dg', j)])
                for tb in range(NTB):
                    ts_ = slice(tb * TB, (tb + 1) * TB)
                    for j in range(31):
                        b.mm(pb[4][:, 0:TB], Dg[j], up[:, j + tb * TB:j + (tb + 1) * TB], start=(j == 0), stop=(j == 30),
                             r=[('Dg', j), uk], w=[('pb', 4)])
                    b.act(ucf[:, ts_], pb[4][:, 0:TB], AF.Identity, bias=cdv[:, ct * 3:ct * 3 + 1], r=[('pb', 4), 'cdv'], w=['ucf'])
                b.cp('act', ucT[:, ct, :], ucf, r=['ucf'], w=[('uc', ct)])
                for tb in range(NTB):
                    ts_ = slice(tb * TB, (tb + 1) * TB)
                    sq_ = tsig[2]
                    b.act(sq_[:, 0:TB], ucf[:, ts_], AF.Square, r=['ucf'], w=[('tsig', 2)])
                    b.mm(pb[2][:, 0:TB], onesf, ucf[:, ts_], r=['cf', 'ucf'], w=[('pb', 2)])
                    b.mm(pb[3][:, 0:TB], onesf, sq_[:, 0:TB], r=['cf', ('tsig', 2)], w=[('pb', 3)])
                    if ct == 0:
                        b.cp('act', S1[:, ts_], pb[2][:, 0:TB], r=[('pb', 2)], w=[('S1', tb)])
                        b.cp('act', S2[:, ts_], pb[3][:, 0:TB], r=[('pb', 3)], w=[('S2', tb)])
                    else:
                        b.tt('dve', S1[:, ts_], S1[:, ts_], pb[2][:, 0:TB], ALU.add, r=[('pb', 2), ('S1', tb)], w=[('S1', tb)])
                        b.tt('dve', S2[:, ts_], S2[:, ts_], pb[3][:, 0:TB], ALU.add, r=[('pb', 3), ('S2', tb)], w=[('S2', tb)])
            s1k = [('S1', tb) for tb in range(NTB)]; s2k = [('S2', tb) for tb in range(NTB)]
            b.ts('dve', S1, S1, 1.0 / D, None, ALU.mult, r=s1k, w=s1k)
            b.tt('dve', ucf, S1, S1, ALU.mult, r=s1k + ['ucf'], w=['ucf'])
            b.stt('dve', S2, S2, 1.0 / D, ucf, ALU.mult, ALU.subtract, r=s2k + ['ucf'], w=s2k)
            b.act(S2, S2, AF.Sqrt, bias=epst[:, 0:1], r=s2k + ['eps'], w=s2k)
            b.S.add('dve', lambda e: e.reciprocal(S2, S2), s2k, s2k)
            for ct in range(KD):
                for tb in range(NTB):
                    ts_ = slice(tb * TB, (tb + 1) * TB)
                    t1 = tsig[(ct * NTB + tb) % 2]
                    k1 = ('tsig', (ct * NTB + tb) % 2)
                    b.tt('dve', t1[:, 0:TB], ucT[:, ct, ts_], S1[:, ts_], ALU.subtract, r=[('uc', ct), ('S1', tb)], w=[k1])
                    b.tt('pool', t1[:, 0:TB], t1[:, 0:TB], S2[:, ts_], ALU.mult, r=[k1, ('S2', tb)], w=[k1])
                    b.act(ucT[:, ct, ts_], t1[:, 0:TB], AF.Silu, bias=cdv[:, ct * 3 + 2:ct * 3 + 3], scale=cdv[:, ct * 3 + 1:ct * 3 + 2],
                          r=[k1, 'cdv'], w=[('uc', ct)])
            uck = [('uc', ct) for ct in range(KD)]
            for f in range(KD):
                wp, wk = wload([wpb_d[:, f * 128:(f + 1) * 128],
                                win_d[:, cfg.GATE_OFF + D + f * 128:cfg.GATE_OFF + D + (f + 1) * 128]])
                for tb in range(NTB):
                    ts_ = slice(tb * TB, (tb + 1) * TB)
                    for k in range(KD):
                        b.mm(pb[0][:, 0:TB], wp[:, k, 0:128], ucT[:, k, ts_], start=(k == 0), stop=(k == KD - 1),
                             r=[wk] + uck, w=[('pb', 0)])
                    for k in range(KD):
                        b.mm(pb[1][:, 0:TB], wp[:, k, 128:256], hT[:, k, ts_], start=(k == 0), stop=(k == KD - 1),
                             r=[wk] + hkeys_all[tb * TPB:(tb + 1) * TPB], w=[('pb', 1)])
                    sg = tsig[tb % 2]
                    b.act(sg[:, 0:TB], pb[1][:, 0:TB], AF.Sigmoid, r=[('pb', 1)], w=[('tsig', tb % 2)])
                    b.tt('dve', sg[:, 0:TB], pb[0][:, 0:TB], sg[:, 0:TB], ALU.mult, r=[('pb', 0), ('tsig', tb % 2)],
                         w=[('tsig', tb % 2)])
                    b.tt('pool', mergedT[:, f, ts_], mergedT[:, f, ts_], sg[:, 0:TB], ALU.add,
                         r=[('mg', f, tb), ('tsig', tb % 2)], w=[('mg', f, tb)])
            S.barrier()
            if STOP < 6:
                return
            make_AB(0, 3, 4, 2)
            make_G(0, 2, 1)
            wA, wkA = wload([wout_d[:, 0:256], wout_d[:, 256:512]] if D >= 512 else [wout_d[:, 0:D]])
            if D > 512:
                wB, wkB = wload([wout_d[:, 512:768], wout_d[:, 768:1024]])
            NH = (D + 511) // 512
            HW = min(512, D)
            O5 = R_B1
            yt = fv(O5, D); x1t = [fv(O5 + D + i * D, D) for i in range(2)]; p1t = [fv(O5 + 3 * D + i * D, D) for i in range(2)]
            tmp5 = fv(O5 + 5 * D, D); hb5 = bv(O5 + 6 * D, D)
            wrt = sbt("wrt", [128, KD * E], BF16)
            wrs = fv(O5 + 7 * D, KD * E).rearrange("p (k n) -> p k n", k=KD)
            b.dma('sp', wrs, wr_d.rearrange("(k p) n -> p k n", p=128), w=['wrs'])
            wrt3 = wrt[:].rearrange("p (k n) -> p k n", k=KD)
            b.cp('pool', wrt3, wrs, r=['wrs'], w=['wrt'])
            brt = sbt("brt", [128, E], F32)
            load_bc(brt[:], br_d[0], 'brt')
            lg = sbt("lg", [128, E], F32); mx8 = sbt("mx8", [128, 8], F32); msk = sbt("msk", [128, E], F32)
            sm = sbt("sm", [128, 4], F32)
            mgk = lambda t: [('mg', f, t // TPB) for f in range(KD)]
            for t in range(NT):
                cs = slice(t * 128, (t + 1) * 128)
                for hf in range(NH):
                    wq = wA if hf == 0 else wB
                    for k in range(KD):
                        b.mm(pb[hf][:, 0:HW], mergedT[:, k, cs], wq[:, k, 0:HW], start=(k == 0), stop=(k == KD - 1),
                             r=mgk(t) + [wkA if hf == 0 else wkB], w=[('pb', hf)])
                    b.cp('act', yt[:, hf * HW:(hf + 1) * HW], pb[hf][:, 0:HW], r=[('pb', hf)], w=['yt'])
                rs, rk = rstd_of(yt, ['yt'], D)
                xi = x1t[t % 2]; pi = p1t[t % 2]
                b.dma('sp', xi, x_d[cs, :], w=[('x1', t % 2)])
                b.dma('sp', pi, pos_d[cs, :], w=[('p1', t % 2)])
                b.tt('pool', xi, xi, pi, ALU.add, r=[('x1', t % 2), ('p1', t % 2)], w=[('x1', t % 2)])
                b.stt('dve', tmp5, yt, rs, M[2][:], ALU.mult, ALU.mult, r=['yt', rk, 'M2'], w=['tmp5'])
                b.tt('dve', xi, xi, tmp5, ALU.add, r=[('x1', t % 2), 'tmp5'], w=[('x1', t % 2)])
                b.dma('sp', x2_d[cs, :], xi, r=[('x1', t % 2)], w=[('x2d', t)])
                rs2, rk2 = rstd_of(xi, [('x1', t % 2)], D)
                b.stt('dve', tmp5, xi, rs2, M[0][:], ALU.mult, ALU.mult, r=[('x1', t % 2), rk2, 'M0'], w=['tmp5'])
                b.tt('dve', hb5, tmp5, M[1][:], ALU.add, r=['tmp5', 'M1'], w=['hb5'])
                for k in range(KD):
                    b.tr(pT[:, k * 128:(k + 1) * 128], hb5[:, k * 128:(k + 1) * 128], identb, r=['hb5', 'identb'], w=['pT'])
                b.cp('act', hT[:, :, cs], pT[:, 0:KD * 128].rearrange("p (k n) -> p k n", k=KD), r=['pT'], w=[('hT', t)])
                for k in range(KD):
                    b.mm(pb[2][:, 0:E], hT[:, k, cs], wrt3[:, k, :], start=(k == 0), stop=(k == KD - 1),
                         r=[('hT', t), 'wrt'], w=[('pb', 2)])
                b.tt('dve', lg[:], pb[2][:, 0:E], brt[:], ALU.add, r=[('pb', 2), 'brt'], w=['lg'])
                b.S.add('dve', lambda e: e.max(mx8[:], lg[:]), ['lg'], ['mx8'])
                b.ts('dve', msk[:], lg[:], mx8[:, 3:4], None, ALU.is_ge, r=['lg', 'mx8'], w=['msk'])
                b.ts('dve', sm[:, 0:1], mx8[:, 0:1], -1.0, None, ALU.mult, r=['mx8'], w=['sm'])
                b.act(lg[:], lg[:], AF.Exp, bias=sm[:, 0:1], r=['lg', 'sm'], w=['lg'])
                b.tt('dve', lg[:], lg[:], msk[:], ALU.mult, r=['lg', 'msk'], w=['lg'])
                b.S.add('dve', lambda e: e.reduce_sum(sm[:, 1:2], lg[:], mybir.AxisListType.X), ['lg'], ['sm'])
                b.S.add('dve', lambda e: e.reciprocal(sm[:, 1:2], sm[:, 1:2]), ['sm'], ['sm'])
                b.ts('dve', Wr[:, t * E:(t + 1) * E], lg[:], sm[:, 1:2], None, ALU.mult, r=['lg', 'sm'], w=[('Wr', t)])
            S.barrier()
            if STOP < 7:
                return
            acc = fv(R_B1, NT * D).rearrange("p (t n) -> p t n", t=NT)
            O6 = O_X
            bdt = AR[0:E, O6:O6 + D]
            b.dma('sp', bdt, bd_d, w=['bdt'])
            wrT = AR[0:E, O6 + D:O6 + D + 128]
            for t in range(NT):
                b.tr(pb[3][0:E, 0:128], Wr[:, t * E:(t + 1) * E], identf, r=[('Wr', t), 'cf'], w=[('pb', 3)])
                b.cp('act', wrT, pb[3][0:E, 0:128], r=[('pb', 3)], w=['wrT'])
                for hf in range(NH):
                    b.mm(pb[hf][:, 0:HW], wrT, bdt[:, hf * HW:(hf + 1) * HW], r=['wrT', 'bdt'], w=[('pb', hf)])
                    b.cp('act' if hf == 0 else 'dve', acc[:, t, hf * HW:(hf + 1) * HW], pb[hf][:, 0:HW], r=[('pb', hf)],
                         w=[('acc', t, hf)])
            S.barrier()
            if STOP < 8:
                return
            CASTENG[0] = 'act'
            actT = bv(O_X, KF * T).rearrange("p (k t) -> p k t", k=KF)
            O_MT = O_X + KF * T // 2
            tGs = [fv(O_MT + i * 512, 512) for i in range(2)]
            tSs = [bv(O_MT + 1024 + i * 256, 512) for i in range(2)]
            tLs = [bv(O_MT + 1536 + i * 256, 512) for i in range(2)]
            assert O_MT + 2048 <= O_WR
            GW = min(256, DFF)
            NPJ = DFF // GW
            FPP = GW // 128
            it = 0
            for e in range(E):
                for j in range(NPJ):
                    wp, wk = wload([wgu_d[e][:, j * GW:(j + 1) * GW], wgu_d[e][:, DFF + j * GW:DFF + (j + 1) * GW]])
                    for fi in range(FPP):
                        ft = j * FPP + fi
                        bg = bgu[:, e * 2 * KF + ft:e * 2 * KF + ft + 1]
                        bl = bgu[:, e * 2 * KF + KF + ft:e * 2 * KF + KF + ft + 1]
                        for tb in range(NTB):
                            ts_ = slice(tb * TB, (tb + 1) * TB)
                            pg = it % 2; pl = 2 + it % 2
                            tG = tGs[it % 2]; tS = tSs[it % 2]; tL = tLs[it % 2]
                            kG = ('tG', it % 2); kS = ('tS', it % 2); kL = ('tL', it % 2)
                            it += 1
                            hk = hkeys_all[tb * TPB:(tb + 1) * TPB]
                            for k in range(KD):
                                b.mm(pb[pg][:, 0:TB], wp[:, k, fi * 128:(fi + 1) * 128], hT[:, k, ts_], start=(k == 0),
                                     stop=(k == KD - 1), r=[wk] + hk, w=[('pb', pg)])
                            for k in range(KD):
                                b.mm(pb[pl][:, 0:TB], wp[:, k, GW + fi * 128:GW + (fi + 1) * 128], hT[:, k, ts_], start=(k == 0),
                                     stop=(k == KD - 1), r=[wk] + hk, w=[('pb', pl)])
                            b.ts('dve', tG[:, 0:TB], pb[pg][:, 0:TB], bg, 7.0, ALU.add, ALU.min, r=[('pb', pg), 'bgu'], w=[kG])
                            b.act(tS[:, 0:TB], tG[:, 0:TB], AF.Sigmoid, scale=1.702, r=[kG], w=[kS])
                            b.act(tL[:, 0:TB], pb[pl][:, 0:TB], AF.Identity, bias=bl, r=[('pb', pl), 'bgu'], w=[kL])
                            b.tt('pool', tG[:, 0:TB], tG[:, 0:TB], tS[:, 0:TB], ALU.mult, r=[kG, kS], w=[kG])
                            b.ts('dve', tL[:, 0:TB], tL[:, 0:TB], -7.0, 7.0, ALU.max, ALU.min, r=[kL], w=[kL])
                            b.stt('dve', actT[:, ft, ts_], tL[:, 0:TB], 1.0, tG[:, 0:TB], ALU.add, ALU.mult, r=[kG, kL],
                                  w=[('aT', ft, tb)])
                for hf in range(NH):
                    if HW == 512:
                        srcs = [wd_d[e][:, hf * 512:hf * 512 + 256], wd_d[e][:, hf * 512 + 256:hf * 512 + 512]]
                    else:
                        srcs = [wd_d[e][:, 0:HW]]
                    wp, wk = wload(srcs)
                    for t in range(NT):
                        cs = slice(t * 128, (t + 1) * 128)
                        pd = 4 + t % 2
                        for k in range(KF):
                            b.mm(pb[pd][:, 0:HW], actT[:, k, cs], wp[:, k, 0:HW], start=(k == 0), stop=(k == KF - 1),
                                 r=[wk] + [('aT', k, t // TPB)], w=[('pb', pd)])
                        b.stt('dve', acc[:, t, hf * HW:(hf + 1) * HW], pb[pd][:, 0:HW], Wr[:, t * E + e:t * E + e + 1],
                              acc[:, t, hf * HW:(hf + 1) * HW], ALU.mult, ALU.add, r=[('pb', pd), ('Wr', t), ('acc', t, hf)],
                              w=[('acc', t, hf)])
            S.barrier()

        phases()
        S.barrier()
        acc = fv(R_B1, NT * D).rearrange("p (t n) -> p t n", t=NT)
        NH = (D + 511) // 512
        make_G(0, 5, 3)
        x2t = [fv(O_X + i * D, D) for i in range(2)]
        tmp7 = fv(O_X + 2 * D, D)
        for t in range(NT):
            cs = slice(t * 128, (t + 1) * 128)
            ak = [('acc', t, hf) for hf in range(NH)]
            rs, rk = rstd_of(acc[:, t, :], ak, D)
            xi = x2t[t % 2]
            b.dma('sp', xi, x2_d[cs, :], r=[('x2d', t)], w=[('x2t', t % 2)])
            b.stt('dve', tmp7, acc[:, t, :], rs, M[2][:], ALU.mult, ALU.mult, r=ak + [rk, 'M2'], w=['tmp7'])
            b.tt('dve', xi, xi, tmp7, ALU.add, r=[('x2t', t % 2), 'tmp7'], w=[('x2t', t % 2)])
            b.dma('sp', out_d[cs, :], xi, r=[('x2t', t % 2)], w=[('out', t)])
        S.emit(nc, es)
    return nc


def pos_table(T, D):
    GRID_W = 64
    rows = T // GRID_W
    row = np.repeat(np.arange(rows, dtype=np.float32), GRID_W)
    col = np.tile(np.arange(GRID_W, dtype=np.float32), rows)
    quarter = D // 4
    omega = (np.float32(10000.0) ** (-np.arange(quarter, dtype=np.float32) / np.float32(quarter))).astype(np.float32)

    def emb(p):
        ang = (p[:, None] * omega[None, :]).astype(np.float32)
        return np.concatenate([np.sin(ang), np.cos(ang)], axis=-1)
    return np.concatenate([emb(row), emb(col)], axis=-1).astype(np.float32)


def fm(v, nt):
    return np.ascontiguousarray(np.asarray(v, np.float32).reshape(nt, 128).T)


def host_maps(inp, cfg):
    D, H, T, TC, E, DFF = cfg.D, cfg.H, cfg.T, cfg.TC, cfg.E, cfg.DFF
    KD = D // 128
    KF = DFF // 128
    f = lambda a: np.ascontiguousarray(np.asarray(a, np.float32))
    cf, cb = host_consts()
    pos = pos_table(T, D)
    ckv = f(inp['conv_kv'])[0]
    cq = f(inp['conv_q'])[0]
    convw = np.zeros((128, 3 * H, 5), np.float32)
    for j in range(2 * H):
        convw[:, j, :] = ckv[:, j * 128:(j + 1) * 128].T
    for j in range(H):
        convw[:, 2 * H + j, :] = cq[:, j * 128:(j + 1) * 128].T
    cdwf = f(inp['conf_dw'])[0]
    cdw = np.zeros((128, KD, 31), np.float32)
    for k in range(KD):
        cdw[:, k, :] = cdwf[:, k * 128:(k + 1) * 128].T
    cdv = np.stack([fm(f(inp['conf_dw_b'])[0], KD), fm(f(inp['conf_ln_g'])[0], KD), fm(f(inp['conf_ln_b'])[0], KD)], axis=-1)
    bguf = f(inp['b_gate_up'])[0]
    bgu = np.zeros((128, E, 2 * KF), np.float32)
    for e in range(E):
        bgu[:, e, :] = bguf[e].reshape(2 * KF, 128).T
    shared = {
        "pos": pos, "w_mod": f(inp['w_mod'])[0], "b_mod2": np.ascontiguousarray(np.stack([f(inp['b_mod'])[0]] * 2)),
        "gvecs": np.ascontiguousarray(np.stack([f(inp['g_pre_mix'])[0], f(inp['g_post_mix'])[0], f(inp['g_pre_ffn'])[0], f(inp['g_post_ffn'])[0]])),
        "w_in": f(inp['w_in'])[0], "convw": convw.reshape(128, -1),
        "ab": np.ascontiguousarray(np.stack([f(inp['a_log'])[0].reshape(-1), f(inp['dt_bias'])[0].reshape(-1)])),
        "gdn_g": f(inp['gdn_norm_g'])[0].reshape(1, 128),
        "w_proj_a": f(inp['w_proj_a'])[0], "w_proj_b": f(inp['w_proj_b'])[0], "w_out": f(inp['w_out'])[0],
        "cdw": cdw.reshape(128, -1), "cdv": np.ascontiguousarray(cdv.reshape(128, -1)),
        "w_router": f(inp['w_router'])[0], "b_router": f(inp['b_router'])[0].reshape(1, E),
        "w_gate_up": f(inp['w_gate_up'])[0], "bgu": bgu.reshape(128, -1),
        "w_down": f(inp['w_down'])[0], "b_down": f(inp['b_down'])[0],
        "cf32": cf, "cb16": cb,
    }
    x = f(inp['x']); c = f(inp['c']); ctx = f(inp['ctx']); cctx = f(inp['c_ctx'])
    maps = []
    for bi in range(x.shape[0]):
        cs = np.stack([c[bi], cctx])
        csT = np.ascontiguousarray(cs.reshape(2, KD, 128).transpose(2, 1, 0).reshape(128, KD * 2))
        m = dict(shared)
        m.update({"x": x[bi], "ctx": ctx[bi], "csT": csT})
        maps.append(m)
    return maps


_NC_CACHE = {}


def kernel(**inputs):
    cfg = Cfg()
    if 'nc' not in _NC_CACHE:
        _NC_CACHE['nc'] = build(cfg)
    nc = _NC_CACHE['nc']
    maps = host_maps(inputs, cfg)
    res = run_bass_kernel_spmd(nc, maps, core_ids=list(range(len(maps))))
    return np.stack([np.asarray(r["out"], np.float32) for r in res.results], axis=0)
```
